# Optimizing a Trainium2 kernel written in Bass

```python
import jax
import jax.numpy as jnp
from jax import lax
import numpy as np

D_MODEL = 1024
BATCH = 8
SEQ = 8192
DEPTH = 2

D_PLE = 256
GRID_W = 64
D_NA = D_MODEL // 2
NA_HEADS = 8
NA_HEAD_DIM = D_NA // NA_HEADS
NA_WIN_H = 8
NA_WIN_W = 16
NA_QBLOCK_W = 16
NA_KBLOCK_W = 32
NA_BIAS_H = 2 * NA_WIN_H - 1
NA_BIAS_W = 2 * NA_WIN_W - 1
D_ML = D_MODEL - D_NA
ML_HEADS = 4
ML_HEAD_DIM = D_ML // ML_HEADS
ML_CONV = 5
ML_CHUNK = 128
N_GATES = 4 * ML_HEADS
D_IN = 3 * D_NA + 4 * D_ML + N_GATES
D_MIX = D_NA + D_ML
N_GROUPS = 4
EXPERTS_PER_GROUP = 8
N_EXPERTS = N_GROUPS * EXPERTS_PER_GROUP
TOP_K = 2
D_EXPERT = 512
MOE_BLOCK = 128
EPS = 1e-6
NEG_INF = -1e30

kernel_name = 'hybrid_na2d_mlstm_hmoe_encoder'


def rmsnorm(x, g):
    xf = x.astype(jnp.float32)
    y = xf * lax.rsqrt(jnp.mean(xf * xf, axis=-1, keepdims=True) + EPS)
    return (y * g.astype(jnp.float32)).astype(x.dtype)


def head_rmsnorm(y, g, n_heads):
    B, S, D = y.shape
    dh = D // n_heads
    return rmsnorm(y.reshape(B, S, n_heads, dh), g.reshape(n_heads, dh)).reshape(B, S, D)


def _clamped_starts(n, win):
    return np.clip(np.arange(n) - win // 2, 0, n - win)


def neighbourhood_attention(q, k, v, rpb):
    B, S, H, Dh = q.shape
    rows = S // GRID_W
    kh = min(NA_WIN_H, rows)
    n_cb = GRID_W // NA_QBLOCK_W
    r = np.arange(rows)
    key_rows = _clamped_starts(rows, kh)[:, None] + np.arange(kh)[None, :]
    col_start = _clamped_starts(GRID_W, NA_WIN_W)
    q_cols = np.arange(GRID_W).reshape(n_cb, NA_QBLOCK_W)
    kb_start = np.minimum(col_start[q_cols[:, 0]], GRID_W - NA_KBLOCK_W)
    key_cols = kb_start[:, None] + np.arange(NA_KBLOCK_W)[None, :]
    cs = col_start[q_cols][:, :, None]
    kc = key_cols[:, None, :]
    in_window = (kc >= cs) & (kc < cs + NA_WIN_W)
    dr = key_rows - r[:, None] + (NA_WIN_H - 1)
    dc = np.clip(kc - q_cols[:, :, None], 1 - NA_WIN_W, NA_WIN_W - 1) + (NA_WIN_W - 1)
    bias = rpb.astype(jnp.float32)[:, dr[:, None, None, :, None], dc[None, :, :, None, :]]
    bias = jnp.where(in_window[None, None, :, :, None, :], bias, NEG_INF)
    qg = q.reshape(B, rows, n_cb, NA_QBLOCK_W, H, Dh)
    ridx = key_rows[:, None, :, None]
    cidx = key_cols[None, :, None, :]
    kg = k.reshape(B, rows, GRID_W, H, Dh)[:, ridx, cidx]
    vg = v.reshape(B, rows, GRID_W, H, Dh)[:, ridx, cidx]
    s = jnp.einsum('brjqhd,brjklhd->bhrjqkl', qg, kg).astype(jnp.float32) * (Dh ** -0.5) + bias[None]
    probs = jax.nn.softmax(s.reshape(B, H, rows, n_cb, NA_QBLOCK_W, kh * NA_KBLOCK_W), axis=-1).astype(v.dtype)
    out = jnp.einsum('bhrjqn,brjnhd->brjqhd', probs, vg.reshape(B, rows, n_cb, kh * NA_KBLOCK_W, H, Dh))
    return out.reshape(B, S, H * Dh)


def mlstm_chunkwise(q, k, v, i_pre, logf):
    B, H, S, Dh = q.shape
    L = ML_CHUNK
    nc = S // L
    qc = q.reshape(B, H, nc, L, Dh)
    kc = k.reshape(B, H, nc, L, Dh)
    vc = v.reshape(B, H, nc, L, Dh)
    ic = i_pre.reshape(B, H, nc, L)
    b = jnp.cumsum(logf.reshape(B, H, nc, L), axis=-1)
    g = b[..., -1]
    a = g[..., None] - b + ic

    def chunk_step(carry, xs):
        C, n, m = carry
        k_c, v_c, a_c, g_c = xs
        m_new = jnp.maximum(g_c + m, jnp.max(a_c, axis=-1))
        w = jnp.exp(a_c - m_new[..., None])
        decay = jnp.exp(g_c + m - m_new)
        C_new = decay[..., None, None] * C + jnp.einsum('bhl,bhld,bhle->bhde', w, v_c, k_c)
        n_new = decay[..., None] * n + jnp.einsum('bhl,bhle->bhe', w, k_c)
        return (C_new, n_new, m_new), (C, n, m)

    init = (jnp.zeros((B, H, Dh, Dh), q.dtype), jnp.zeros((B, H, Dh), q.dtype), jnp.zeros((B, H), q.dtype))
    xs = (jnp.moveaxis(kc, 2, 0), jnp.moveaxis(vc, 2, 0), jnp.moveaxis(a, 2, 0), jnp.moveaxis(g, 2, 0))
    _, (C_prev, n_prev, m_prev) = lax.scan(chunk_step, init, xs)
    C_prev = jnp.moveaxis(C_prev, 0, 2)
    n_prev = jnp.moveaxis(n_prev, 0, 2)
    m_prev = jnp.moveaxis(m_prev, 0, 2)
    lower = np.tril(np.ones((L, L), dtype=bool))
    log_d = jnp.where(lower, b[..., :, None] - b[..., None, :] + ic[..., None, :], -jnp.inf)
    log_inter = b + m_prev[..., None]
    m_out = jnp.maximum(log_inter, jnp.max(log_d, axis=-1))
    s = jnp.einsum('bhcld,bhcsd->bhcls', qc, kc) * jnp.exp(log_d - m_out[..., None])
    w_inter = jnp.exp(log_inter - m_out)
    num = jnp.einsum('bhcls,bhcsd->bhcld', s, vc) + w_inter[..., None] * jnp.einsum('bhcde,bhcle->bhcld', C_prev, qc)
    den = jnp.sum(s, axis=-1) + w_inter * jnp.einsum('bhce,bhcle->bhcl', n_prev, qc)
    h = num / jnp.maximum(jnp.abs(den), jnp.exp(-m_out))[..., None]
    return h.reshape(B, H, S, Dh)


def hybrid_mixer(a, w_in, b_gate, conv_w, conv_b, rpb, g_na, g_ml, w_out):
    B, S, _ = a.shape
    z = jnp.einsum('bsd,de->bse', a, w_in)
    q_na, k_na, v_na, qk_ml, v_ml, o_ml, gate_pre = jnp.split(
        z, [D_NA, 2 * D_NA, 3 * D_NA, 3 * D_NA + 2 * D_ML, 3 * D_NA + 3 * D_ML, 3 * D_NA + 4 * D_ML], axis=-1)
    shape_na = (B, S, NA_HEADS, NA_HEAD_DIM)
    y_na = neighbourhood_attention(q_na.reshape(shape_na), k_na.reshape(shape_na), v_na.reshape(shape_na), rpb)
    qk = lax.conv_general_dilated(qk_ml, conv_w[:, None, :].astype(qk_ml.dtype), window_strides=(1,),
                                  padding=((ML_CONV // 2, ML_CONV // 2),),
                                  dimension_numbers=('NWC', 'WIO', 'NWC'), feature_group_count=2 * D_ML)
    qk = jax.nn.silu(qk + conv_b)
    q_ml, k_ml = jnp.split(qk, 2, axis=-1)

    def to_heads(t):
        return t.reshape(B, S, ML_HEADS, ML_HEAD_DIM).transpose(0, 2, 1, 3).astype(jnp.float32)

    q_h = to_heads(q_ml) * (ML_HEAD_DIM ** -0.5)
    k_h = to_heads(k_ml)
    v_h = to_heads(v_ml)
    gates = (gate_pre.astype(jnp.float32) + b_gate.astype(jnp.float32)).reshape(B, S, 4, ML_HEADS).transpose(2, 0, 3, 1)
    i_fw, f_fw, i_bw, f_bw = gates[0], gates[1], gates[2], gates[3]
    h_fw = mlstm_chunkwise(q_h, k_h, v_h, i_fw, jax.nn.log_sigmoid(f_fw))
    flip = lambda t: jnp.flip(t, axis=2)
    h_bw = flip(mlstm_chunkwise(flip(q_h), flip(k_h), flip(v_h), flip(i_bw), flip(jax.nn.log_sigmoid(f_bw))))
    h_ml = (h_fw + h_bw).transpose(0, 2, 1, 3).reshape(B, S, D_ML).astype(a.dtype)
    y_ml = jax.nn.sigmoid(o_ml) * head_rmsnorm(h_ml, g_ml, ML_HEADS)
    y = jnp.concatenate([head_rmsnorm(y_na, g_na, NA_HEADS), y_ml], axis=-1)
    return jnp.einsum('bse,ed->bsd', y, w_out)


def routed_experts(t, expert_id, weights, w_gate, w_up, w_down):
    T, D = t.shape
    P = T * TOP_K
    n_blocks = P // MOE_BLOCK + N_EXPERTS
    flat_e = expert_id.reshape(-1).astype(jnp.int32)
    flat_tok = jnp.arange(P, dtype=jnp.int32) // TOP_K
    flat_w = weights.reshape(-1)
    order = jnp.argsort(flat_e)
    se, stok, sw = flat_e[order], flat_tok[order], flat_w[order]
    counts = jnp.bincount(flat_e, length=N_EXPERTS)
    padded = ((counts + MOE_BLOCK - 1) // MOE_BLOCK) * MOE_BLOCK
    pad_end = jnp.cumsum(padded)
    pad_start = pad_end - padded
    start = jnp.cumsum(counts) - counts
    dest = pad_start[se] + (jnp.arange(P, dtype=jnp.int32) - start[se])
    buf_tok = jnp.zeros((n_blocks * MOE_BLOCK,), jnp.int32).at[dest].set(stok)
    buf_w = jnp.zeros((n_blocks * MOE_BLOCK,), t.dtype).at[dest].set(sw.astype(t.dtype))
    block_e = jnp.minimum(jnp.searchsorted(pad_end, jnp.arange(n_blocks, dtype=jnp.int32) * MOE_BLOCK, side='right'),
                          N_EXPERTS - 1)

    def block_ffn(args):
        tok, wt, e = args
        xb = t[tok]
        hid = jax.nn.silu(xb @ w_gate[e]) * (xb @ w_up[e])
        return (hid @ w_down[e]) * wt[:, None]

    yb = lax.map(block_ffn, (buf_tok.reshape(n_blocks, MOE_BLOCK), buf_w.reshape(n_blocks, MOE_BLOCK), block_e))
    return jax.ops.segment_sum(yb.reshape(-1, D), buf_tok, num_segments=T)


def hierarchical_moe(x, w_rg, b_rg, w_re, b_re, w_gate, w_up, w_down):
    B, S, D = x.shape
    t = x.reshape(B * S, D)
    T = t.shape[0]
    p_group = jax.nn.softmax((t @ w_rg).astype(jnp.float32) + b_rg.astype(jnp.float32), axis=-1)
    pg_top, g_idx = lax.top_k(p_group, 1)
    e_logits = ((t @ w_re).astype(jnp.float32) + b_re.astype(jnp.float32)).reshape(T, N_GROUPS, EXPERTS_PER_GROUP)
    e_logits = e_logits[jnp.arange(T), g_idx[:, 0]]
    pe_top, e_local = lax.top_k(jax.nn.softmax(e_logits, axis=-1), TOP_K)
    weights = pg_top * pe_top / jnp.sum(pe_top, axis=-1, keepdims=True)
    expert_id = g_idx * EXPERTS_PER_GROUP + e_local
    y = routed_experts(t, expert_id, weights, w_gate, w_up, w_down)
    return y.reshape(B, S, D)


def per_layer_embedding(h, p_i, g, w_ple, w_pg):
    gate = jax.nn.sigmoid(jnp.einsum('bsd,de->bse', rmsnorm(h, g), w_pg))
    return jnp.einsum('bsp,pd->bsd', p_i, w_ple) * gate


def setup_inputs(seed: int = 0) -> dict:
    key = jax.random.key(seed)
    ks = jax.random.split(key, 26)
    f32 = jnp.float32

    def nrm(k, shape, scale):
        return jax.random.normal(k, shape, f32) * scale

    def gain(k, shape):
        return 1.0 + nrm(k, shape, 0.02)

    f_bias = jnp.linspace(3.0, 6.0, ML_HEADS, dtype=f32)
    b_gate = jnp.concatenate([
        nrm(ks[2], (DEPTH, ML_HEADS), 0.1),
        f_bias + nrm(ks[3], (DEPTH, ML_HEADS), 0.1),
        nrm(ks[4], (DEPTH, ML_HEADS), 0.1),
        f_bias + nrm(ks[5], (DEPTH, ML_HEADS), 0.1)], axis=-1)
    return {
        'x': nrm(ks[0], (BATCH, SEQ, D_MODEL), 1.0),
        'p': nrm(ks[1], (DEPTH, BATCH, SEQ, D_PLE), 1.0),
        'w_in': nrm(ks[6], (DEPTH, D_MODEL, D_IN), D_MODEL ** -0.5),
        'b_gate': b_gate,
        'conv_w': nrm(ks[7], (DEPTH, ML_CONV, 2 * D_ML), ML_CONV ** -0.5),
        'conv_b': nrm(ks[8], (DEPTH, 2 * D_ML), 0.02),
        'rpb': nrm(ks[9], (DEPTH, NA_HEADS, NA_BIAS_H, NA_BIAS_W), 0.1),
        'g_na': gain(ks[10], (DEPTH, D_NA)),
        'g_ml': gain(ks[11], (DEPTH, D_ML)),
        'w_out': nrm(ks[12], (DEPTH, D_MIX, D_MODEL), D_MIX ** -0.5),
        'g_mix': gain(ks[13], (DEPTH, D_MODEL)),
        'g_moe': gain(ks[14], (DEPTH, D_MODEL)),
        'w_route_group': nrm(ks[15], (DEPTH, D_MODEL, N_GROUPS), D_MODEL ** -0.5),
        'b_route_group': nrm(ks[16], (DEPTH, N_GROUPS), 0.01),
        'w_route_expert': nrm(ks[17], (DEPTH, D_MODEL, N_EXPERTS), D_MODEL ** -0.5),
        'b_route_expert': nrm(ks[18], (DEPTH, N_EXPERTS), 0.01),
        'w_exp_gate': nrm(ks[19], (DEPTH, N_EXPERTS, D_MODEL, D_EXPERT), D_MODEL ** -0.5),
        'w_exp_up': nrm(ks[20], (DEPTH, N_EXPERTS, D_MODEL, D_EXPERT), D_MODEL ** -0.5),
        'w_exp_down': nrm(ks[21], (DEPTH, N_EXPERTS, D_EXPERT, D_MODEL), D_EXPERT ** -0.5),
        'g_ple': gain(ks[22], (DEPTH, D_MODEL)),
        'w_ple': nrm(ks[23], (DEPTH, D_PLE, D_MODEL), D_PLE ** -0.5),
        'w_ple_gate': nrm(ks[24], (DEPTH, D_MODEL, D_MODEL), D_MODEL ** -0.5),
        'g_final': gain(ks[25], (D_MODEL,)),
    }


def reference(x, p, w_in, b_gate, conv_w, conv_b, rpb, g_na, g_ml, w_out, g_mix, g_moe,
              w_route_group, b_route_group, w_route_expert, b_route_expert,
              w_exp_gate, w_exp_up, w_exp_down, g_ple, w_ple, w_ple_gate, g_final):
    h = x
    for i in range(DEPTH):
        h = h + hybrid_mixer(rmsnorm(h, g_mix[i]), w_in[i], b_gate[i], conv_w[i], conv_b[i], rpb[i],
                             g_na[i], g_ml[i], w_out[i])
        h = h + hierarchical_moe(rmsnorm(h, g_moe[i]), w_route_group[i], b_route_group[i],
                                 w_route_expert[i], b_route_expert[i],
                                 w_exp_gate[i], w_exp_up[i], w_exp_down[i])
        h = h + per_layer_embedding(h, p[i], g_ple[i], w_ple[i], w_ple_gate[i])
    return rmsnorm(h, g_final)
```

```python
import numpy as np
from contextlib import ExitStack
import concourse.bass as bass
import concourse.mybir as mybir
from concourse.bass_utils import run_bass_kernel_spmd

F32 = mybir.dt.float32
BF16 = mybir.dt.bfloat16
AF = mybir.ActivationFunctionType
ALU = mybir.AluOpType
AX = mybir.AxisListType

S = 8192
D = 1024
NT = 64
DEPTH = 2
D_IN = 3600
EPS = 1e-6
NEG = -1e30


class Buf:
    __slots__ = ("last_w", "readers")

    def __init__(self):
        self.last_w = None
        self.readers = []


class KB:
    COMPUTE = ("pe", "dve", "act", "pool")

    def __init__(self, nc, stack, n_dma_sems=(("sp", 20), ("pool", 24), ("bg", 48))):
        self.nc = nc
        self.e = dict(pe=nc.tensor, dve=nc.vector, act=nc.scalar, pool=nc.gpsimd, sp=nc.sync)
        self.csem = {k: stack.enter_context(nc.semaphore(f"c_{k}")) for k in self.COMPUTE}
        self.cnt = {k: 0 for k in self.COMPUTE}
        self.seen = {k: {} for k in self.e}
        self.seen["bg"] = self.seen["pool"]
        self.dsem = {}
        self.dpos = {}
        for q, n in n_dma_sems:
            self.dsem[q] = [[stack.enter_context(nc.semaphore(f"d_{q}{i}")), 0, None] for i in range(n)]
            self.dpos[q] = 0

    def _wait(self, eng, tok):
        if tok is None:
            return
        sem, val = tok
        key = id(sem)
        if self.seen[eng].get(key, 0) >= val:
            return
        self.e[eng].wait_ge(sem, val)
        self.seen[eng][key] = val

    def _deps(self, eng, r, w):
        own = self.csem.get(eng)
        best = {}

        def add(t):
            k = id(t[0])
            if k not in best or best[k][1] < t[1]:
                best[k] = t
        for b in r:
            if b.last_w is not None:
                add(b.last_w)
        for b in w:
            if b.last_w is not None and b.last_w[0] is not own:
                add(b.last_w)
            for t in b.readers:
                if t[0] is not own:
                    add(t)
        for t in best.values():
            self._wait(eng, t)

    def _commit(self, tok, r, w):
        for b in r:
            b.readers.append(tok)
            if len(b.readers) > 48:
                best = {}
                for t in b.readers:
                    k = id(t[0])
                    if k not in best or best[k][1] < t[1]:
                        best[k] = t
                b.readers = list(best.values())
        for b in w:
            b.last_w = tok
            b.readers = []

    def op(self, eng, fn, r=(), w=()):
        self._deps(eng, r, w)
        ins = fn()
        self.cnt[eng] += 1
        ins.then_inc(self.csem[eng], 1)
        tok = (self.csem[eng], self.cnt[eng])
        self._commit(tok, r, w)
        return tok

    def dma(self, q, out, in_, r=(), w=(), ring=None, **kw):
        self._deps(q, r, w)
        rq = ring or q
        ring = self.dsem[rq]
        slot = ring[self.dpos[rq] % len(ring)]
        self.dpos[rq] += 1
        if slot[2] is not None:
            self._wait(q, slot[2])
        ins = self.e[q].dma_start(out=out, in_=in_, **kw)
        slot[1] += 16
        ins.then_inc(slot[0], 16)
        tok = (slot[0], slot[1])
        slot[2] = tok
        self._commit(tok, r, w)
        return tok

    def idma(self, out, out_off, in_, in_off, r=(), w=(), bc=None):
        q = "pool"
        self._deps(q, r, w)
        ring = self.dsem[q]
        slot = ring[self.dpos[q] % len(ring)]
        self.dpos[q] += 1
        if slot[2] is not None:
            self._wait(q, slot[2])
        ins = self.nc.gpsimd.indirect_dma_start(out=out, out_offset=out_off, in_=in_, in_offset=in_off)
        slot[1] += 16
        ins.then_inc(slot[0], 16)
        tok = (slot[0], slot[1])
        slot[2] = tok
        self._commit(tok, r, w)
        return tok

    def barrier(self):
        toks = [(self.csem[k], self.cnt[k]) for k in self.COMPUTE if self.cnt[k] > 0]
        for q in self.dsem:
            for s in self.dsem[q]:
                if s[2] is not None:
                    toks.append(s[2])
        for eng in self.e:
            for t in toks:
                self._wait(eng, t)


class Ctx:
    pass


def _consts():
    ii = np.arange(128)
    c = {}
    c["ident"] = np.eye(128, dtype=np.float32)
    c["antiid"] = np.eye(128, dtype=np.float32)[::-1].copy()
    c["ufw"] = (ii[:, None] <= ii[None, :]).astype(np.float32)
    c["ubw"] = (ii[:, None] >= ii[None, :]).astype(np.float32)
    c["ones"] = np.ones((128, 128), np.float32)
    c["base8"] = (np.arange(8)[None, :] * 128 + ii[:, None]).astype(np.float32)
    c["jB"] = np.broadcast_to((np.arange(32) * 512).astype(np.float32)[None, :], (128, 32)).copy()
    c["bst"] = np.broadcast_to((np.arange(64) * 512).astype(np.float32)[None, :], (128, 64)).copy()
    return c


def _na_tables(rpb):
    H = 8
    out = np.empty((5, 128, H, 5, 128), np.float32)
    kp = np.arange(128)
    ql = np.arange(128)
    for ti, u in enumerate((0, 1, 2, 62, 63)):
        t0 = min(max(u - 2, 0), 59)
        r = 2 * u + ql // 64
        c = ql % 64
        rs = np.clip(r - 4, 0, 120)
        cs = np.clip(c - 8, 0, 48)
        for j in range(5):
            kr = 2 * (t0 + j) + kp // 64
            kc = kp % 64
            inwin = ((kr[:, None] >= rs[None, :]) & (kr[:, None] < rs[None, :] + 8) &
                     (kc[:, None] >= cs[None, :]) & (kc[:, None] < cs[None, :] + 16))
            dr = np.clip(kr[:, None] - r[None, :] + 7, 0, 14)
            dc = np.clip(kc[:, None] - c[None, :], -15, 15) + 15
            b = rpb[:, dr, dc]
            b = np.where(inwin[None], b, np.float32(NEG))
            out[ti, :, :, j, :] = b.transpose(1, 0, 2)
    return out


def build_program(debug=None, stop_after=None):
    nc = bass.Bass("TRN2", target_bir_lowering=False)
    dt_in = lambda name, shape, dt=F32: nc.dram_tensor(name, list(shape), dt, kind="ExternalInput").ap()
    dt_out = lambda name, shape, dt=F32: nc.dram_tensor(name, list(shape), dt, kind="ExternalOutput").ap()
    dt_tmp = lambda name, shape, dt=F32: nc.dram_tensor(name, list(shape), dt).ap()

    I = Ctx()
    I.x = dt_in("x", [S, D])
    I.p = dt_in("p", [DEPTH, S, 256])
    I.w_in = dt_in("w_in", [DEPTH, D, D_IN])
    I.b_gate = dt_in("b_gate", [DEPTH, 16])
    I.conv_w = dt_in("conv_w", [DEPTH, 5, 1024])
    I.conv_b = dt_in("conv_b", [DEPTH, 1024])
    I.natab = dt_in("natab", [DEPTH, 5, 128, 8 * 5 * 128])
    I.g_na = dt_in("g_na", [DEPTH, 512])
    I.g_ml = dt_in("g_ml", [DEPTH, 512])
    I.w_out = dt_in("w_out", [DEPTH, D, D])
    I.g_mix = dt_in("g_mix", [DEPTH, D])
    I.g_moe = dt_in("g_moe", [DEPTH, D])
    I.w_rt = dt_in("w_rt", [DEPTH, D, 36])
    I.b_rt = dt_in("b_rt", [DEPTH, 36])
    I.w_eg = dt_in("w_exp_gate", [DEPTH * 32 * 128, 8 * 512])
    I.w_eu = dt_in("w_exp_up", [DEPTH * 32 * 128, 8 * 512])
    I.w_ed = dt_in("w_exp_down", [DEPTH * 32 * 128, 4 * D])
    I.g_ple = dt_in("g_ple", [DEPTH, D])
    I.w_ple = dt_in("w_ple", [DEPTH, 256, D])
    I.w_pg = dt_in("w_ple_gate", [DEPTH, D, D])
    I.g_final = dt_in("g_final", [1, D])
    I.ident = dt_in("ident", [128, 128])
    I.antiid = dt_in("antiid", [128, 128])
    I.ufw = dt_in("ufw", [128, 128])
    I.ubw = dt_in("ubw", [128, 128])
    I.ones = dt_in("ones", [128, 128])
    I.base8 = dt_in("base8", [128, 8])
    I.jB = dt_in("jB", [128, 32])
    I.bst = dt_in("bst", [128, 64])
    out = dt_out("out", [S, D])

    T = Ctx()
    T.h = dt_tmp("h_scr", [S, D])
    T.qnaT = dt_tmp("qnaT", [512, S], BF16)
    T.knaT = dt_tmp("knaT", [512, S], BF16)
    T.vna = dt_tmp("vna", [S, 520], BF16)
    T.qkT = dt_tmp("qkT", [1024, S], BF16)
    T.vml = dt_tmp("vml", [S, 512], BF16)
    T.sigo = dt_tmp("sigo", [S, 512], BF16)
    T.gatesP = dt_tmp("gatesP", [128, NT, 16])
    T.ymix = dt_tmp("ymix", [S, D], BF16)
    T.sc = dt_tmp("sc_small", [16, 256])
    T.Xn = dt_tmp("Xn", [S, D], BF16)
    T.Xs = dt_tmp("Xs", [64 * 512, D], BF16)
    T.Ys = dt_tmp("Ys", [64 * 512, D], BF16)
    T.Wbf = [dt_tmp(f"Wbf{m}", [32 * 128, 4096], BF16) for m in range(3)]
    dbg = {}
    if debug:
        for name, shape, dtt in debug:
            dbg[name] = dt_out("dbg_" + name, shape, dtt)

    with ExitStack() as st0:
        k = KB(nc, st0)
        uid = [0]

        def sb(stack, name, shape, dt):
            uid[0] += 1
            return stack.enter_context(nc.sbuf_tensor(f"{name}_{uid[0]}", list(shape), dt))

        def ps(stack, name, shape, dt):
            uid[0] += 1
            return stack.enter_context(nc.psum_tensor(f"{name}_{uid[0]}", list(shape), dt))

        C = Ctx()
        C.identf = sb(st0, "identf", [128, 128], F32)
        C.identb = sb(st0, "identb", [128, 128], BF16)
        C.antif = sb(st0, "antif", [128, 128], F32)
        C.ufw = sb(st0, "c_ufw", [128, 128], F32)
        C.ubw = sb(st0, "c_ubw", [128, 128], F32)
        C.ones = sb(st0, "c_ones", [128, 128], F32)
        C.base8 = sb(st0, "c_base8", [128, 8], F32)
        C.jB = sb(st0, "c_jB", [128, 32], F32)
        C.bst = sb(st0, "c_bst", [128, 64], F32)
        C.B = Buf()
        for t, src in ((C.identf, I.ident), (C.antif, I.antiid), (C.ufw, I.ufw), (C.ubw, I.ubw), (C.ones, I.ones),
                       (C.base8, I.base8), (C.jB, I.jB), (C.bst, I.bst)):
            k.dma("sp", t[:], src, w=[C.B])
        k.op("dve", lambda: nc.vector.tensor_copy(out=C.identb[:], in_=C.identf[:]), r=[C.B], w=[C.B])
        k.barrier()

        Bh = [Buf() for _ in range(NT)]
        env = dict(nc=nc, k=k, I=I, T=T, C=C, sb=sb, ps=ps, Bh=Bh, out=out, dbg=dbg)

        for layer in range(DEPTH):
            src_h = I.x if layer == 0 else T.h
            stage_inproj(env, layer, src_h)
            k.barrier()
            if stop_after == ("A", layer):
                break
            stage_na(env, layer)
            k.barrier()
            if stop_after == ("B", layer):
                break
            stage_mlstm(env, layer)
            k.barrier()
            if stop_after == ("C", layer):
                break
            stage_tail(env, layer, src_h, stop_after)
            k.barrier()
            if stop_after is not None and stop_after[1] == layer:
                break
        for name in dbg:
            src = dict(z_vna=T.vna, z_qnaT=T.qnaT, z_knaT=T.knaT, z_qkT=T.qkT, z_vml=T.vml, z_sigo=T.sigo,
                       z_gatesP=T.gatesP, ymix=T.ymix, h=T.h).get(name)
            if src is not None:
                k.dma("sp", dbg[name], src)
        k.barrier()
    return nc


def norm_pre(env, h_sb, Bh_sb, g_bc, Bg, tmp):
    nc, k = env["nc"], env["k"]
    k.op("act", lambda: nc.scalar.activation(out=tmp["sq"][:], in_=h_sb, func=AF.Square, accum_out=tmp["ss"][:]),
         r=[Bh_sb], w=[tmp["Bsq"], tmp["Bss"]])
    k.op("act", lambda: nc.scalar.activation(out=tmp["rs"][:], in_=tmp["ss"][:], func=AF.Ln, bias=EPS, scale=1.0 / D),
         r=[tmp["Bss"]], w=[tmp["Brs"]])
    k.op("act", lambda: nc.scalar.activation(out=tmp["rs"][:], in_=tmp["rs"][:], func=AF.Exp, scale=-0.5), r=[tmp["Brs"]], w=[tmp["Brs"]])
    k.op("dve", lambda: nc.vector.scalar_tensor_tensor(out=tmp["a"][:], in0=h_sb, scalar=tmp["rs"][:], in1=g_bc[:],
                                                        op0=ALU.mult, op1=ALU.mult),
         r=[Bh_sb, tmp["Brs"], Bg], w=[tmp["Ba"]])


def norm_post(env, aT, BaT, col0, tmp):
    nc, k, C = env["nc"], env["k"], env["C"]
    for kc in range(8):
        k.op("pe", lambda: nc.tensor.transpose(out=tmp["pT"][:, kc * 128:(kc + 1) * 128],
                                               in_=tmp["a"][:, kc * 128:(kc + 1) * 128], identity=C.identb[:]),
             r=[tmp["Ba"], C.B], w=[tmp["BpT"]])
    k.op("act", lambda: nc.scalar.copy(out=aT[:, :, col0:col0 + 128],
                                       in_=tmp["pT"][:].rearrange("p (c t) -> p c t", c=8)),
         r=[tmp["BpT"]], w=[BaT])


def norm_tile(env, stk_tiles, h_sb, Bh_sb, g_bc, Bg, aT, BaT, col0, tmp):
    norm_pre(env, h_sb, Bh_sb, g_bc, Bg, tmp)
    norm_post(env, aT, BaT, col0, tmp)


def make_norm_tmp(env, stk, tag):
    sb, ps = env["sb"], env["ps"]
    t = {}
    t["sq"] = sb(stk, f"nsq{tag}", [128, D], F32)
    t["ss"] = sb(stk, f"nss{tag}", [128, 1], F32)
    t["rs"] = sb(stk, f"nrs{tag}", [128, 1], F32)
    t["a"] = sb(stk, f"na{tag}", [128, D], BF16)
    t["pT"] = ps(stk, f"npT{tag}", [128, D], BF16)
    for n in ("Bsq", "Bss", "Brs", "Ba", "BpT"):
        t[n] = Buf()
    return t


def stage_inproj(env, layer, src_h):
    nc, k, I, T, C, sb, ps, Bh = (env[n] for n in ("nc", "k", "I", "T", "C", "sb", "ps", "Bh"))
    with ExitStack() as stk:
        W = sb(stk, "A_w", [128, 8, D_IN], BF16)
        BW = Buf()
        wsrc = I.w_in[layer].rearrange("(c p) n -> p c n", p=128)
        for kc in range(8):
            k.dma("pool", W[:, kc, :], wsrc[:, kc, :], w=[BW])
        gbc = sb(stk, "A_g", [128, D], F32)
        Bg = Buf()
        k.dma("sp", gbc[:], I.g_mix[layer:layer + 1, :].broadcast_to([128, D]), w=[Bg])
        bgate = sb(stk, "A_bg", [128, 16], F32)
        k.dma("sp", bgate[:], I.b_gate[layer:layer + 1, :].broadcast_to([128, 16]), w=[Bg])
        ntmps = [make_norm_tmp(env, stk, f"A{i}") for i in range(2)]
        hs = [sb(stk, f"A_h{i}", [128, D], F32) for i in range(8)]
        Bhs = [Buf() for _ in range(8)]
        aT = [sb(stk, f"A_aT{i}", [128, 8, 512], BF16) for i in range(2)]
        BaT = [Buf(), Buf()]
        pF = [ps(stk, f"A_pF{i}", [128, 512], F32) for i in range(2)]
        BpF = [Buf(), Buf()]
        pTk = [ps(stk, f"A_pT{i}", [128, 512], F32) for i in range(3)]
        BpTk = [Buf() for _ in range(3)]
        zF = [sb(stk, f"A_zF{i}", [128, 512], BF16) for i in range(4)]
        BzF = [Buf() for _ in range(4)]
        zT = [sb(stk, f"A_zT{i}", [128, 512], BF16) for i in range(4)]
        BzT = [Buf() for _ in range(4)]
        gt = [sb(stk, f"A_gt{i}", [128, 16], F32) for i in range(2)]
        Bgt = [Buf(), Buf()]
        zV = [sb(stk, f"A_zV{i}", [128, 8, 65], BF16) for i in range(2)]
        BzV = [Buf(), Buf()]
        for i in range(2):
            k.op("pool", lambda: nc.gpsimd.memset(zV[i][:], 1.0), w=[BzV[i]])
        fm = []
        for c in range(4):
            fm.append((c * 128, T.qnaT, c * 128, 0.125))
        for c in range(4):
            fm.append((512 + c * 128, T.knaT, c * 128, 1.0))
        for c in range(8):
            fm.append((1536 + c * 128, T.qkT, c * 128, 1.0))
        nF = 0
        nTk = 0
        nz = 0

        def a_loads(g):
            for j in range(4):
                t = g * 4 + j
                i8 = (g % 2) * 4 + j
                k.dma("sp", hs[i8][:], src_h[t * 128:(t + 1) * 128, :], r=[Bh[t]], w=[Bhs[i8]])

        def a_pre(g, j):
            i8 = (g % 2) * 4 + j
            norm_pre(env, hs[i8][:], Bhs[i8], gbc, Bg, ntmps[j % 2])

        def a_post(g, j):
            norm_post(env, aT[g % 2], BaT[g % 2], j * 128, ntmps[j % 2])

        a_loads(0)
        for j in range(4):
            a_pre(0, j)
            a_post(0, j)
        for g in range(NT // 4):
            a_t = aT[g % 2]
            Ba = BaT[g % 2]
            nxt = g + 1 < NT // 4
            if nxt:
                a_loads(g + 1)
            for (zc, dst, drow, scale) in fm:
                pf = pF[nF % 2]
                Bp = BpF[nF % 2]
                nF += 1
                for kc in range(8):
                    k.op("pe", lambda: nc.tensor.matmul(out=pf[:], lhsT=W[:, kc, zc:zc + 128], rhs=a_t[:, kc, :],
                                                        start=(kc == 0), stop=(kc == 7)), r=[BW, Ba], w=[Bp])
                zf = zF[nz % 4]
                Bz = BzF[nz % 4]
                nz += 1
                k.op("act", lambda: nc.scalar.activation(out=zf[:], in_=pf[:], func=AF.Copy, scale=scale), r=[Bp], w=[Bz])
                k.dma("sp", dst[drow:drow + 128, g * 512:(g + 1) * 512], zf[:], r=[Bz])
            for j in range(4):
                t = g * 4 + j
                if nxt:
                    a_pre(g + 1, j)
                for which, (zc, n) in enumerate(((1024, 512), (2560, 512), (3072, 512), (3584, 16))):
                    pt = pTk[nTk % 3]
                    Bp = BpTk[nTk % 3]
                    nTk += 1
                    for kc in range(8):
                        k.op("pe", lambda: nc.tensor.matmul(out=pt[:, 0:n], lhsT=a_t[:, kc, j * 128:(j + 1) * 128],
                                                            rhs=W[:, kc, zc:zc + n], start=(kc == 0), stop=(kc == 7)),
                             r=[BW, Ba], w=[Bp])
                    if which == 0:
                        zv, Bzv = zV[t % 2], BzV[t % 2]
                        k.op("dve", lambda: nc.vector.tensor_copy(out=zv[:, :, 0:64], in_=pt[:].rearrange("p (h d) -> p h d", d=64)),
                             r=[Bp], w=[Bzv])
                        k.dma("sp", T.vna[t * 128:(t + 1) * 128, :], zv[:].rearrange("p h e -> p (h e)"), r=[Bzv])
                    elif which < 3:
                        zt = zT[(nz) % 4]
                        Bz = BzT[(nz) % 4]
                        nz += 1
                        if which == 2:
                            k.op("act", lambda: nc.scalar.activation(out=zt[:], in_=pt[:], func=AF.Sigmoid), r=[Bp], w=[Bz])
                        else:
                            k.op("dve", lambda: nc.vector.tensor_copy(out=zt[:], in_=pt[:]), r=[Bp], w=[Bz])
                        dst = (T.vna, T.vml, T.sigo)[which]
                        k.dma("sp", dst[t * 128:(t + 1) * 128, :], zt[:], r=[Bz])
                    else:
                        g_t = gt[t % 2]
                        Bg_t = Bgt[t % 2]
                        k.op("dve", lambda: nc.vector.tensor_tensor(out=g_t[:], in0=pt[:, 0:16], in1=bgate[:], op=ALU.add),
                             r=[Bp, Bg], w=[Bg_t])
                        k.dma("sp", T.gatesP[:, t, :], g_t[:], r=[Bg_t])
                if nxt:
                    a_post(g + 1, j)


def stage_na(env, layer):
    nc, k, I, T, C, sb, ps = (env[n] for n in ("nc", "k", "I", "T", "C", "sb", "ps"))
    with ExitStack() as stk:
        tabI = sb(stk, "N_tabI", [128, 8 * 640], F32)
        tabE = [sb(stk, f"N_tabE{i}", [128, 8 * 640], F32) for i in range(2)]
        BtI, BtE = Buf(), [Buf(), Buf()]
        k.dma("sp", tabI[:], I.natab[layer, 2], w=[BtI])
        gna = sb(stk, "N_g", [128, 512], F32)
        Bg = Buf()
        k.dma("sp", gna[:], I.g_na[layer:layer + 1, :].broadcast_to([128, 512]), w=[Bg])
        NBUF = 3
        Kt = [sb(stk, f"N_K{i}", [128, 4, 640], BF16) for i in range(NBUF)]
        Qt = [sb(stk, f"N_Q{i}", [128, 4, 128], BF16) for i in range(NBUF)]
        Vt = [sb(stk, f"N_V{i}", [128, 5, 8, 65], BF16) for i in range(NBUF)]
        BK, BQ, BV = ([Buf() for _ in range(NBUF)] for _ in range(3))
        pS = [ps(stk, f"N_pS{i}", [128, 1536], F32) for i in range(2)]
        BpS = [Buf(), Buf()]
        pO = ps(stk, "N_pO", [128, 2, 512], F32)
        BpO = Buf()
        sc = [sb(stk, f"N_sc{i}", [128, 1280], F32) for i in range(2)]
        pr = [sb(stk, f"N_pr{i}", [128, 1280], BF16) for i in range(2)]
        Bsc, Bpr = [Buf(), Buf()], [Buf(), Buf()]
        rden = sb(stk, "N_rden", [128, 8], F32)
        y = sb(stk, "N_y", [128, 8, 64], F32)
        ysq = sb(stk, "N_ysq", [128, 8, 64], F32)
        ssq = sb(stk, "N_ssq", [128, 8], F32)
        yb = [sb(stk, f"N_yb{i}", [128, 512], BF16) for i in range(2)]
        Brd, By, Bysq, Bssq, Byb = Buf(), Buf(), Buf(), Buf(), [Buf(), Buf()]
        qsrc = T.qnaT.rearrange("(c p) s -> p c s", p=128)
        ksrc = T.knaT.rearrange("(c p) s -> p c s", p=128)
        tabs = {}

        def loads(u):
            t0 = min(max(u - 2, 0), 59)
            b = u % NBUF
            k.dma("sp", Kt[b][:], ksrc[:, :, t0 * 128:t0 * 128 + 640], w=[BK[b]])
            k.dma("sp", Qt[b][:], qsrc[:, :, u * 128:(u + 1) * 128], w=[BQ[b]])
            k.dma("sp", Vt[b][:].rearrange("p j h e -> p j (h e)"),
                  T.vna[t0 * 128:t0 * 128 + 640, :].rearrange("(j p) n -> p j n", p=128), w=[BV[b]])
            if u in (0, 1, 62, 63):
                ti = {0: 0, 1: 1, 62: 3, 63: 4}[u]
                k.dma("sp", tabE[u % 2][:], I.natab[layer, ti], w=[BtE[u % 2]])
                tabs[u] = (tabE[u % 2], BtE[u % 2])
            else:
                tabs[u] = (tabI, BtI)

        def scores(i):
            u, un = divmod(i, 4)
            if un == 0:
                loads(u)
            b = u % NBUF
            for hh in range(2):
                h = un * 2 + hh
                pb = (h % 2) * 64
                for j in range(5):
                    k.op("pe", lambda: nc.tensor.matmul(out=pS[i % 2][:, hh * 640 + j * 128: hh * 640 + (j + 1) * 128],
                                                        lhsT=Kt[b][pb:pb + 64, h // 2, j * 128:(j + 1) * 128],
                                                        rhs=Qt[b][pb:pb + 64, h // 2, :], start=True, stop=True),
                         r=[BK[b], BQ[b]], w=[BpS[i % 2]])

        def softmax_pv(i):
            u, un = divmod(i, 4)
            b = u % NBUF
            tab, Bt = tabs[u]
            k.op("dve", lambda: nc.vector.tensor_tensor(out=sc[i % 2][:], in0=pS[i % 2][:, 0:1280],
                                                        in1=tab[:, un * 1280:(un + 1) * 1280], op=ALU.add),
                 r=[BpS[i % 2], Bt], w=[Bsc[i % 2]])
            k.op("act", lambda: nc.scalar.activation(out=pr[i % 2][:], in_=sc[i % 2][:], func=AF.Exp), r=[Bsc[i % 2]], w=[Bpr[i % 2]])
            for hh in range(2):
                h = un * 2 + hh
                for j in range(5):
                    k.op("pe", lambda: nc.tensor.matmul(out=pO[:, h // 4, (h % 4) * 65:(h % 4) * 65 + 65],
                                                        lhsT=pr[i % 2][:, hh * 640 + j * 128: hh * 640 + (j + 1) * 128],
                                                        rhs=Vt[b][:, j, h, :], start=(j == 0), stop=(j == 4)),
                         r=[Bpr[i % 2], BV[b]], w=[BpO])

        def epilogue(u):
            o4 = pO[:, :, 0:260].rearrange("p a (h e) -> p a h e", e=65)
            k.op("dve", lambda: nc.vector.reciprocal(out=rden[:].rearrange("p (a h) -> p a h", a=2), in_=o4[:, :, :, 64]),
                 r=[BpO], w=[Brd])
            k.op("dve", lambda: nc.vector.tensor_tensor(out=y[:].rearrange("p (a h) d -> p a h d", a=2), in0=o4[:, :, :, 0:64],
                                                        in1=rden[:].rearrange("p (a h) -> p a h", a=2).unsqueeze(3).broadcast_to([128, 2, 4, 64]),
                                                        op=ALU.mult), r=[BpO, Brd], w=[By])
            k.op("pool", lambda: nc.gpsimd.tensor_tensor(out=ysq[:], in0=y[:], in1=y[:], op=ALU.mult), r=[By], w=[Bysq])
            k.op("dve", lambda: nc.vector.tensor_reduce(out=ssq[:], in_=ysq[:], axis=AX.X, op=ALU.add), r=[Bysq], w=[Bssq])
            k.op("act", lambda: nc.scalar.activation(out=ssq[:], in_=ssq[:], func=AF.Ln, bias=EPS, scale=1.0 / 64), r=[Bssq], w=[Bssq])
            k.op("act", lambda: nc.scalar.activation(out=ssq[:], in_=ssq[:], func=AF.Exp, scale=-0.5), r=[Bssq], w=[Bssq])
            k.op("pool", lambda: nc.gpsimd.tensor_tensor(out=y[:], in0=y[:], in1=ssq[:].unsqueeze(2).broadcast_to([128, 8, 64]), op=ALU.mult),
                 r=[By, Bssq], w=[By])
            yb_, Bb = yb[u % 2], Byb[u % 2]
            k.op("pool", lambda: nc.gpsimd.tensor_tensor(out=yb_[:], in0=y[:].rearrange("p h d -> p (h d)"), in1=gna[:], op=ALU.mult),
                 r=[By, Bg], w=[Bb])
            k.dma("sp", T.ymix[u * 128:(u + 1) * 128, 0:512], yb_[:], r=[Bb])

        N = NT * 4
        scores(0)
        for i in range(N):
            if i + 1 < N:
                scores(i + 1)
            softmax_pv(i)
            if i % 4 == 3:
                epilogue(i // 4)


def stage_mlstm(env, layer):
    nc, k, I, T, C, sb, ps = (env[n] for n in ("nc", "k", "I", "T", "C", "sb", "ps"))
    QS = 128 ** -0.5
    with ExitStack() as stk:
        E1 = [sb(stk, f"M_E1{d}", [128, 256], F32) for d in range(2)]
        E2 = [sb(stk, f"M_E2{d}", [128, 256], F32) for d in range(2)]
        EM = [sb(stk, f"M_EM{d}", [128, 256], F32) for d in range(2)]
        EG = [sb(stk, f"M_EG{d}", [128, 256], F32) for d in range(2)]
        BE = Buf()
        with ExitStack() as s2:
            G = sb(s2, "M_G", [128, 64, 16], F32)
            BG = Buf()
            k.dma("sp", G[:], T.gatesP, w=[BG])
            ex = sb(s2, "M_ex", [128, 256], F32)
            SP = sb(s2, "M_SP", [128, 256], F32)
            U = sb(s2, "M_U", [128, 256], F32)
            Bs = sb(s2, "M_Bs", [128, 256], F32)
            Gs = sb(s2, "M_Gs", [128, 256], F32)
            uT = sb(s2, "M_uT", [128, 2, 128], F32)
            cmT = sb(s2, "M_cmT", [128, 2, 128], F32)
            gT = sb(s2, "M_gT", [128, 2, 128], F32)
            amx = sb(s2, "M_amx", [128, 2], F32)
            ngT = sb(s2, "M_ngT", [128, 2], F32)
            cmr = sb(s2, "M_cmr", [128, 256], F32)
            cm = sb(s2, "M_cm", [128, 256], F32)
            MP = sb(s2, "M_MP", [128, 256], F32)
            mx = sb(s2, "M_mx", [128, 256], F32)
            am4 = sb(s2, "M_am4", [4, 64], F32)
            gg4 = sb(s2, "M_gg4", [4, 64], F32)
            m4 = sb(s2, "M_m4", [4, 64], F32)
            mp4 = sb(s2, "M_mp4", [4, 64], F32)
            pA = ps(s2, "M_pA", [128, 256], F32)
            pB = ps(s2, "M_pB", [128, 256], F32)
            pC = ps(s2, "M_pC", [128, 2, 128], F32)
            pD = ps(s2, "M_pD", [128, 2, 128], F32)
            Bx = {n: Buf() for n in ("ex", "SP", "U", "Bs", "Gs", "uT", "cmT", "gT", "amx", "ngT", "cmr", "cm", "MP", "mx",
                                     "am4", "gg4", "m4", "mp4", "pA", "pB", "pC", "pD", "sc")}
            for d in range(2):
                Iv = G[:, :, 8 * d:8 * d + 4]
                Fv = G[:, :, 8 * d + 4:8 * d + 8]
                v3 = lambda t: t[:].rearrange("p (c h) -> p c h", h=4)
                Ud = C.ufw if d == 0 else C.ubw
                idm = C.identf if d == 0 else C.antif
                k.op("act", lambda: nc.scalar.activation(out=v3(ex), in_=Fv, func=AF.Exp, scale=-1.0), r=[BG], w=[Bx["ex"]])
                k.op("act", lambda: nc.scalar.activation(out=SP[:], in_=ex[:], func=AF.Ln, bias=1.0), r=[Bx["ex"]], w=[Bx["SP"]])
                k.op("pe", lambda: nc.tensor.matmul(out=pA[:], lhsT=Ud[:], rhs=SP[:], start=True, stop=True), r=[Bx["SP"], C.B], w=[Bx["pA"]])
                k.op("pe", lambda: nc.tensor.matmul(out=pB[:], lhsT=C.ones[:], rhs=SP[:], start=True, stop=True), r=[Bx["SP"], C.B], w=[Bx["pB"]])
                k.op("dve", lambda: nc.vector.tensor_tensor(out=v3(U), in0=pA[:].rearrange("p (c h) -> p c h", h=4), in1=Iv, op=ALU.add),
                     r=[Bx["pA"], BG], w=[Bx["U"]])
                k.op("dve", lambda: nc.vector.tensor_copy(out=Bs[:], in_=pA[:]), r=[Bx["pA"]], w=[Bx["Bs"]])
                k.op("dve", lambda: nc.vector.tensor_copy(out=Gs[:], in_=pB[:]), r=[Bx["pB"]], w=[Bx["Gs"]])
                k.op("act", lambda: nc.scalar.activation(out=EG[d][:], in_=Gs[:], func=AF.Exp, scale=-1.0), r=[Bx["Gs"]], w=[BE])
                k.op("act", lambda: nc.scalar.activation(out=E1[d][:], in_=U[:], func=AF.Exp), r=[Bx["U"]], w=[BE])
                for blk in range(2):
                    k.op("pe", lambda: nc.tensor.transpose(out=pC[:, blk, :], in_=U[:, blk * 128:(blk + 1) * 128], identity=idm[:]),
                         r=[Bx["U"], C.B], w=[Bx["pC"]])
                    k.op("pe", lambda: nc.tensor.transpose(out=pD[:, blk, :], in_=Gs[:, blk * 128:(blk + 1) * 128], identity=C.identf[:]),
                         r=[Bx["Gs"], C.B], w=[Bx["pD"]])
                k.op("dve", lambda: nc.vector.tensor_copy(out=uT[:], in_=pC[:]), r=[Bx["pC"]], w=[Bx["uT"]])
                k.op("dve", lambda: nc.vector.tensor_copy(out=gT[:], in_=pD[:]), r=[Bx["pD"]], w=[Bx["gT"]])
                for blk in range(2):
                    k.op("dve", lambda: nc.vector.tensor_tensor_scan(out=cmT[:, blk, :], data0=uT[:, blk, :], data1=uT[:, blk, :],
                                                                      initial=-3.0e38, op0=ALU.max, op1=ALU.max),
                         r=[Bx["uT"]], w=[Bx["cmT"]])
                k.op("dve", lambda: nc.vector.tensor_tensor(out=amx[:], in0=cmT[:, :, 127], in1=gT[:, :, 0], op=ALU.subtract),
                     r=[Bx["cmT"], Bx["gT"]], w=[Bx["amx"]])
                k.op("dve", lambda: nc.vector.tensor_scalar(out=ngT[:], in0=gT[:, :, 0], scalar1=-1.0, scalar2=None, op0=ALU.mult),
                     r=[Bx["gT"]], w=[Bx["ngT"]])
                for blk in range(2):
                    k.dma("sp", T.sc[d * 4 + 0:d * 4 + 1, blk * 128:(blk + 1) * 128].rearrange("o n -> n o"), amx[:, blk:blk + 1],
                          r=[Bx["amx"]], w=[Bx["sc"]])
                    k.dma("sp", T.sc[d * 4 + 1:d * 4 + 2, blk * 128:(blk + 1) * 128].rearrange("o n -> n o"), ngT[:, blk:blk + 1],
                          r=[Bx["ngT"]], w=[Bx["sc"]])
                k.dma("sp", am4[:], T.sc[d * 4 + 0].rearrange("(c h) -> h c", h=4), r=[Bx["sc"]], w=[Bx["am4"]],
                      allow_slow_non_contiguous=True)
                k.dma("sp", gg4[:], T.sc[d * 4 + 1].rearrange("(c h) -> h c", h=4), r=[Bx["sc"]], w=[Bx["gg4"]],
                      allow_slow_non_contiguous=True)
                if d == 0:
                    k.op("dve", lambda: nc.vector.tensor_tensor_scan(out=m4[:], data0=gg4[:], data1=am4[:], initial=0.0,
                                                                      op0=ALU.add, op1=ALU.max),
                         r=[Bx["gg4"], Bx["am4"]], w=[Bx["m4"]])
                    k.op("dve", lambda: nc.vector.memset(mp4[:, 0:1], 0.0), w=[Bx["mp4"]])
                    k.op("dve", lambda: nc.vector.tensor_copy(out=mp4[:, 1:64], in_=m4[:, 0:63]), r=[Bx["m4"]], w=[Bx["mp4"]])
                else:
                    k.op("dve", lambda: nc.vector.tensor_tensor(out=m4[:, 63:64], in0=gg4[:, 63:64], in1=am4[:, 63:64], op=ALU.max),
                         r=[Bx["gg4"], Bx["am4"]], w=[Bx["m4"]])
                    for c in range(62, -1, -1):
                        k.op("dve", lambda: nc.vector.scalar_tensor_tensor(out=m4[:, c:c + 1], in0=m4[:, c + 1:c + 2], scalar=gg4[:, c:c + 1],
                                                                            in1=am4[:, c:c + 1], op0=ALU.add, op1=ALU.max),
                             r=[Bx["m4"], Bx["gg4"], Bx["am4"]], w=[Bx["m4"]])
                    k.op("dve", lambda: nc.vector.memset(mp4[:, 63:64], 0.0), w=[Bx["mp4"]])
                    k.op("dve", lambda: nc.vector.tensor_copy(out=mp4[:, 0:63], in_=m4[:, 1:64]), r=[Bx["m4"]], w=[Bx["mp4"]])
                k.dma("sp", T.sc[d * 4 + 2].rearrange("(c h) -> h c", h=4), mp4[:], r=[Bx["mp4"]], w=[Bx["sc"]],
                      allow_slow_non_contiguous=True)
                k.dma("sp", MP[:], T.sc[d * 4 + 2:d * 4 + 3, :].broadcast_to([128, 256]), r=[Bx["sc"]], w=[Bx["MP"]])
                for blk in range(2):
                    k.op("pe", lambda: nc.tensor.transpose(out=pA[:, blk * 128:(blk + 1) * 128], in_=cmT[:, blk, :], identity=C.identf[:]),
                         r=[Bx["cmT"], C.B], w=[Bx["pA"]])
                if d == 0:
                    k.op("dve", lambda: nc.vector.tensor_copy(out=cm[:], in_=pA[:]), r=[Bx["pA"]], w=[Bx["cm"]])
                else:
                    k.op("dve", lambda: nc.vector.tensor_copy(out=cmr[:], in_=pA[:]), r=[Bx["pA"]], w=[Bx["cmr"]])
                    k.op("pe", lambda: nc.tensor.matmul(out=pB[:], lhsT=C.antif[:], rhs=cmr[:], start=True, stop=True),
                         r=[Bx["cmr"], C.B], w=[Bx["pB"]])
                    k.op("dve", lambda: nc.vector.tensor_copy(out=cm[:], in_=pB[:]), r=[Bx["pB"]], w=[Bx["cm"]])
                k.op("dve", lambda: nc.vector.tensor_tensor(out=mx[:], in0=MP[:], in1=cm[:], op=ALU.max), r=[Bx["MP"], Bx["cm"]], w=[Bx["mx"]])
                k.op("act", lambda: nc.scalar.activation(out=E2[d][:], in_=mx[:], func=AF.Exp, scale=-1.0), r=[Bx["mx"]], w=[BE])
                k.op("dve", lambda: nc.vector.tensor_tensor(out=mx[:], in0=Bs[:], in1=mx[:], op=ALU.subtract), r=[Bx["Bs"], Bx["mx"]], w=[Bx["mx"]])
                k.op("act", lambda: nc.scalar.activation(out=EM[d][:], in_=mx[:], func=AF.Exp), r=[Bx["mx"]], w=[BE])
            k.barrier()
        cw = sb(stk, "M_cw", [128, 8, 5], F32)
        cb = sb(stk, "M_cb", [128, 8], F32)
        gml = sb(stk, "M_gml", [128, 512], F32)
        Bcw = Buf()
        for j in range(5):
            k.dma("sp", cw[:, :, j], I.conv_w[layer, j].rearrange("(n p) -> p n", p=128), w=[Bcw], allow_slow_non_contiguous=True)
        k.dma("sp", cb[:], I.conv_b[layer].rearrange("(n p) -> p n", p=128), w=[Bcw], allow_slow_non_contiguous=True)
        k.dma("sp", gml[:], I.g_ml[layer:layer + 1, :].broadcast_to([128, 512]), w=[Bcw])
        Bmask = Buf()
        qT = sb(stk, "M_qT", [128, 2, S], BF16)
        kT = sb(stk, "M_kT", [128, 2, S], BF16)
        BqT = [[Buf() for _ in range(8)] for _ in range(2)]
        BkT = [[Buf() for _ in range(8)] for _ in range(2)]
        va = sb(stk, "M_va", [128, 64, 2, 129], BF16)
        Bva = Buf()
        hacc = sb(stk, "M_hacc", [128, 64, 256], F32)
        Bh = [Buf() for _ in range(64)]
        xin = [sb(stk, f"M_xin{i}", [128, 1028], BF16) for i in range(2)]
        dg = sb(stk, "M_dg", [128, 4, 5, 128], BF16)
        Bdg = Buf()
        Bxin = [Buf(), Buf()]
        V = nc.vector
        Cs = sb(stk, "M_Cs", [128, 4, 129], F32)
        Cb = sb(stk, "M_Cb", [128, 4, 129], BF16)
        BCs, BCb = Buf(), Buf()
        mask4 = sb(stk, "M_mask4", [128, 4, 128], F32)
        for u_ in range(4):
            k.op("act", lambda: nc.scalar.mul(out=mask4[:, u_, :], in_=(C.ufw if u_ < 2 else C.ubw)[:], mul=QS), r=[C.B], w=[Bmask])
        ST = [sb(stk, f"M_ST{i}", [128, 4, 128], BF16) for i in range(2)]
        vp = [sb(stk, f"M_vp{i}", [128, 2, 2, 129], BF16) for i in range(2)]
        nd = [sb(stk, f"M_nd{i}", [128, 2, 2, 129], F32) for i in range(2)]
        kk = [sb(stk, f"M_kk{i}", [128, 4, 128], BF16) for i in range(2)]
        dd = [sb(stk, f"M_dd{i}", [128, 8], F32) for i in range(2)]
        htmp = [sb(stk, "M_htmp", [128, 2, 2, 128], F32)] * 2
        BST, Bvp, Bnd, Bkk, Bdd = ([Buf(), Buf()] for _ in range(5))
        Bhtmp = [Buf()] * 2
        P1 = [ps(stk, f"M_P1{i}", [128, 4, 128], F32) for i in range(2)]
        PT = [ps(stk, f"M_PT{i}", [128, 4, 128], BF16) for i in range(2)]
        P2 = [ps(stk, f"M_P2{d}", [128, 2, 129], F32) for d in range(2)]
        P3 = [ps(stk, f"M_P3{d}", [128, 2, 129], F32) for d in range(2)]
        BP1, BPT, BP2, BP3 = ([Buf(), Buf()] for _ in range(4))
        fsq = sb(stk, "M_fsq", [128, 256], F32)
        fss = sb(stk, "M_fss", [128, 2], F32)
        fy = sb(stk, "M_fy", [128, 256], F32)
        fso = [sb(stk, f"M_fso{i}", [128, 256], BF16) for i in range(2)]
        fyb = [sb(stk, f"M_fyb{i}", [128, 256], BF16) for i in range(2)]
        Bfsq, Bfss, Bfy, Bfso, Bfyb = Buf(), Buf(), Buf(), [Buf(), Buf()], [Buf(), Buf()]
        vsrc = T.vml.rearrange("(c p) (h d) -> p c h d", p=128, d=128)
        ncv = 0
        ncp = 0
        un = 0
        for hp in range(2):
            k.op("pool", lambda: nc.gpsimd.memset(va[:], 1.0), w=[Bva])
            for hh in range(2):
                k.dma("sp", va[:, :, hh, 0:128], vsrc[:, :, hp * 2 + hh, :], w=[Bva])
            for isk_ in range(2):
                for hh_ in range(2):
                    n_ = isk_ * 4 + hp * 2 + hh_
                    for j_ in range(5):
                        k.op("pool", lambda: nc.gpsimd.tensor_scalar(out=dg[:, isk_ * 2 + hh_, j_, :], in0=C.identf[:], scalar1=cw[:, n_, j_:j_ + 1],
                                                                     scalar2=None, op0=ALU.mult), r=[C.B, Bcw], w=[Bdg])
            for isk in range(2):
                for hh in range(2):
                    n = isk * 4 + hp * 2 + hh
                    dstT = kT if isk else qT
                    for pc in range(8):
                        xi, Bxi = xin[ncv % 2], Bxin[ncv % 2]
                        ncv += 1
                        lo = pc * 1024 - 2
                        hi = pc * 1024 + 1026
                        o0 = 0
                        if pc == 0:
                            k.op("pool", lambda: nc.gpsimd.memset(xi[:, 0:2], 0.0), w=[Bxi])
                            lo, o0 = 0, 2
                        if pc == 7:
                            k.op("pool", lambda: nc.gpsimd.memset(xi[:, 1026:1028], 0.0), w=[Bxi])
                            hi = S
                        k.dma("sp", xi[:, o0:o0 + (hi - lo)], T.qkT[n * 128:(n + 1) * 128, lo:hi], w=[Bxi])
                        Bd = (BkT if isk else BqT)[hh][pc]
                        for sub in range(2):
                            pcv = P1[ncp % 2][:].rearrange("p a b -> p (a b)")
                            Bpcv = BP1[ncp % 2]
                            ncp += 1
                            for j in range(5):
                                k.op("pe", lambda: nc.tensor.matmul(out=pcv, lhsT=dg[:, isk * 2 + hh, j, :], rhs=xi[:, sub * 512 + j:sub * 512 + j + 512],
                                                                    start=(j == 0), stop=(j == 4)), r=[Bdg, Bxi], w=[Bpcv])
                            t0_ = pc * 1024 + sub * 512
                            k.op("act", lambda: nc.scalar.activation(out=dstT[:, hh, t0_:t0_ + 512], in_=pcv, func=AF.Silu,
                                                                     bias=cb[:, n:n + 1]), r=[Bpcv, Bcw], w=[Bd])
            for step in range(64):
                i2 = step % 2
                first = (step == 0)
                cc = (step, 63 - step)
                cols = [cc[d] * 4 + hp * 2 for d in range(2)]
                csl = [slice(cc[d] * 128, (cc[d] + 1) * 128) for d in range(2)]
                Bq = [[BqT[hh][cc[d] // 8] for hh in range(2)] for d in range(2)]
                Bk = [[BkT[hh][cc[d] // 8] for hh in range(2)] for d in range(2)]
                for d in range(2):
                    for hh in range(2):
                        k.op("pe", lambda: nc.tensor.matmul(out=P1[i2][:, d * 2 + hh, :], lhsT=kT[:, hh, csl[d]], rhs=qT[:, hh, csl[d]],
                                                            start=True, stop=True), r=[Bq[d][hh], Bk[d][hh]], w=[BP1[i2]])
                k.op("dve", lambda: V.tensor_tensor(out=ST[i2][:], in0=P1[i2][:], in1=mask4[:], op=ALU.mult), r=[BP1[i2], Bmask], w=[BST[i2]])
                for d in range(2):
                    k.op("pool", lambda: nc.gpsimd.tensor_tensor(out=vp[i2][:, d, :, :], in0=va[:, cc[d], :, :],
                                                                 in1=E1[d][:, cols[d]:cols[d] + 2].unsqueeze(2).broadcast_to([128, 2, 129]),
                                                                 op=ALU.mult), r=[Bva, BE], w=[Bvp[i2]])
                for d in range(2):
                    for hh in range(2):
                        k.op("pe", lambda: nc.tensor.matmul(out=P2[d][:, hh, :], lhsT=ST[i2][:, d * 2 + hh, :], rhs=vp[i2][:, d, hh, :],
                                                            start=True, stop=first), r=[BST[i2], Bvp[i2]], w=[BP2[d]])
                        if not first:
                            k.op("pe", lambda: nc.tensor.matmul(out=P2[d][:, hh, :], lhsT=qT[:, hh, csl[d]], rhs=Cb[:, d * 2 + hh, :],
                                                                start=False, stop=True), r=[Bq[d][hh], BCb], w=[BP2[d]])
                for d in range(2):
                    k.op("dve", lambda: V.tensor_tensor(out=nd[i2][:, d, :, :], in0=P2[d][:],
                                                        in1=E2[d][:, cols[d]:cols[d] + 2].unsqueeze(2).broadcast_to([128, 2, 129]), op=ALU.mult),
                         r=[BP2[d], BE], w=[Bnd[i2]])
                den = nd[i2][:, :, :, 128]
                dd_ = dd[i2]
                k.op("dve", lambda: V.scalar_tensor_tensor(out=dd_[:, 0:4].rearrange("p (a b) -> p a b", a=2), in0=den, scalar=-1.0, in1=den,
                                                            op0=ALU.mult, op1=ALU.max), r=[Bnd[i2]], w=[Bdd[i2]])
                for d in range(2):
                    k.op("dve", lambda: V.tensor_tensor(out=dd_[:, 4 + 2 * d:6 + 2 * d], in0=dd_[:, 2 * d:2 * d + 2],
                                                        in1=EM[d][:, cols[d]:cols[d] + 2], op=ALU.max), r=[Bdd[i2], BE], w=[Bdd[i2]])
                k.op("dve", lambda: V.reciprocal(out=dd_[:, 0:4], in_=dd_[:, 4:8]), r=[Bdd[i2]], w=[Bdd[i2]])
                for d in range(2):
                    c = cc[d]
                    hdst = hacc[:, c, :].rearrange("p (h e) -> p h e", h=2)
                    rdb = dd_[:, 2 * d:2 * d + 2].unsqueeze(2).broadcast_to([128, 2, 128])
                    if (d == 0 and c < 32) or (d == 1 and c >= 32):
                        k.op("dve", lambda: V.tensor_tensor(out=hdst, in0=nd[i2][:, d, :, 0:128], in1=rdb, op=ALU.mult),
                             r=[Bnd[i2], Bdd[i2]], w=[Bh[c]])
                    else:
                        k.op("pool", lambda: nc.gpsimd.tensor_tensor(out=htmp[i2][:, d, :, :], in0=nd[i2][:, d, :, 0:128], in1=rdb, op=ALU.mult),
                             r=[Bnd[i2], Bdd[i2]], w=[Bhtmp[i2]])
                        k.op("pool", lambda: nc.gpsimd.tensor_tensor(out=hdst, in0=hdst, in1=htmp[i2][:, d, :, :], op=ALU.add),
                             r=[Bhtmp[i2], Bh[c]], w=[Bh[c]])
                if step == 63:
                    continue
                for d in range(2):
                    for hh in range(2):
                        k.op("pe", lambda: nc.tensor.transpose(out=PT[i2][:, d * 2 + hh, :], in_=kT[:, hh, csl[d]], identity=C.identb[:]),
                             r=[Bk[d][hh], C.B], w=[BPT[i2]])
                k.op("act", lambda: nc.scalar.copy(out=kk[i2][:], in_=PT[i2][:]), r=[BPT[i2]], w=[Bkk[i2]])
                for d in range(2):
                    for hh in range(2):
                        k.op("pe", lambda: nc.tensor.matmul(out=P3[d][:, hh, :], lhsT=kk[i2][:, d * 2 + hh, :], rhs=vp[i2][:, d, hh, :],
                                                            start=True, stop=True), r=[Bkk[i2], Bvp[i2]], w=[BP3[d]])
                for d in range(2):
                    egb = EG[d][:, cols[d]:cols[d] + 2].unsqueeze(2).broadcast_to([128, 2, 129])
                    if first:
                        k.op("dve", lambda: V.tensor_tensor(out=Cs[:, 2 * d:2 * d + 2, :], in0=P3[d][:], in1=egb, op=ALU.mult),
                             r=[BP3[d], BE], w=[BCs])
                    else:
                        k.op("dve", lambda: V.tensor_tensor(out=Cs[:, 2 * d:2 * d + 2, :], in0=Cs[:, 2 * d:2 * d + 2, :], in1=P3[d][:], op=ALU.add),
                             r=[BP3[d], BCs], w=[BCs])
                        k.op("dve", lambda: V.tensor_tensor(out=Cs[:, 2 * d:2 * d + 2, :], in0=Cs[:, 2 * d:2 * d + 2, :], in1=egb, op=ALU.mult),
                             r=[BCs, BE], w=[BCs])
                k.op("act", lambda: nc.scalar.mul(out=Cb[:], in_=Cs[:], mul=QS), r=[BCs], w=[BCb])
            for c in range(64):
                so, Bso = fso[c % 2], Bfso[c % 2]
                ybf, Byb = fyb[c % 2], Bfyb[c % 2]
                k.dma("sp", so[:], T.sigo[c * 128:(c + 1) * 128, hp * 256:(hp + 1) * 256], w=[Bso])
                k.op("pool", lambda: nc.gpsimd.tensor_tensor(out=fsq[:], in0=hacc[:, c, :], in1=hacc[:, c, :], op=ALU.mult), r=[Bh[c]], w=[Bfsq])
                k.op("dve", lambda: nc.vector.tensor_reduce(out=fss[:], in_=fsq[:].rearrange("p (h d) -> p h d", h=2), axis=AX.X, op=ALU.add),
                     r=[Bfsq], w=[Bfss])
                k.op("act", lambda: nc.scalar.activation(out=fss[:], in_=fss[:], func=AF.Sqrt, bias=EPS, scale=1.0 / 128), r=[Bfss], w=[Bfss])
                k.op("dve", lambda: nc.vector.reciprocal(out=fss[:], in_=fss[:]), r=[Bfss], w=[Bfss])
                k.op("dve", lambda: nc.vector.tensor_tensor(out=fy[:].rearrange("p (h d) -> p h d", h=2), in0=hacc[:, c, :].rearrange("p (h d) -> p h d", h=2),
                                                            in1=fss[:].unsqueeze(2).broadcast_to([128, 2, 128]), op=ALU.mult),
                     r=[Bh[c], Bfss], w=[Bfy])
                k.op("pool", lambda: nc.gpsimd.tensor_tensor(out=fy[:], in0=fy[:], in1=gml[:, hp * 256:(hp + 1) * 256], op=ALU.mult),
                     r=[Bfy, Bcw], w=[Bfy])
                k.op("dve", lambda: nc.vector.tensor_tensor(out=ybf[:], in0=fy[:], in1=so[:], op=ALU.mult), r=[Bfy, Bso], w=[Byb])
                k.dma("sp", T.ymix[c * 128:(c + 1) * 128, 512 + hp * 256:512 + (hp + 1) * 256], ybf[:], r=[Byb])
            k.barrier()


def stage_tail(env, layer, src_h, stop_after):
    nc, k, I, T, C, sb, ps, Bh, out, dbg = (env[n] for n in ("nc", "k", "I", "T", "C", "sb", "ps", "Bh", "out", "dbg"))
    BS = 512
    NB = 64
    last = (layer == DEPTH - 1)
    V = nc.vector
    weg, weu, wed = T.Wbf
    with ExitStack() as stk:
        OH1 = sb(stk, "T_OH1", [128, NT, 32], F32)
        OH2 = sb(stk, "T_OH2", [128, NT, 32], F32)
        W12 = sb(stk, "T_W12", [128, NT, 2], F32)
        DI = sb(stk, "T_DI", [128, NT, 2], mybir.dt.int32)
        IG = sb(stk, "T_IG", [128, NB], mybir.dt.int32)
        BOH = Buf()
        with ExitStack() as s2:
            Wo = sb(s2, "T_Wo", [128, 8, D], BF16)
            BWo = Buf()
            k.dma("pool", Wo[:], I.w_out[layer].rearrange("(c p) n -> p c n", p=128), w=[BWo])
            gmoe = sb(s2, "T_gmoe", [128, D], F32)
            brt = sb(s2, "T_brt", [128, 36], F32)
            wr = sb(s2, "T_wr", [128, 8, 36], BF16)
            Bg = Buf()
            k.dma("sp", gmoe[:], I.g_moe[layer:layer + 1, :].broadcast_to([128, D]), w=[Bg])
            k.dma("sp", brt[:], I.b_rt[layer:layer + 1, :].broadcast_to([128, 36]), w=[Bg])
            k.dma("pool", wr[:], I.w_rt[layer].rearrange("(c p) n -> p c n", p=128), w=[Bg])
            for m, src in enumerate((I.w_eg, I.w_eu, I.w_ed)):
                for e in range(32):
                    r0 = (layer * 32 + e) * 128
                    k.dma("pool", T.Wbf[m][e * 128:(e + 1) * 128, :], src[r0:r0 + 128, :], ring="bg")
            ntmps = [make_norm_tmp(env, s2, f"T{i}") for i in range(2)]
            po = [ps(s2, f"T_po{i}", [128, 512], F32) for i in range(2)]
            Bpo = [Buf(), Buf()]
            prs = [ps(s2, f"T_pr{i}", [128, 8, 36], F32) for i in range(2)]
            Bprs = [Buf(), Buf()]
            ym = [sb(s2, f"T_ym{i}", [128, D], BF16) for i in range(2)]
            hb = [sb(s2, f"T_hb{i}", [128, D], F32) for i in range(2)]
            h1 = [sb(s2, f"T_h1{i}", [128, D], F32) for i in range(2)]
            Bym, Bhb, Bh1 = [Buf(), Buf()], [Buf(), Buf()], [Buf(), Buf()]
            ymTs = [sb(s2, f"T_ymT{i}", [128, 8, 128], BF16) for i in range(2)]
            xTts = [sb(s2, f"T_xTt{i}", [128, 8, 128], BF16) for i in range(2)]
            BymTs, BxTts = [Buf(), Buf()], [Buf(), Buf()]
            Ls = [sb(s2, f"T_L{i}", [128, 8, 36], F32) for i in range(2)]
            rts = [sb(s2, f"T_rt{i}", [128, 8 * 56], F32) for i in range(2)]
            Brts = [Buf(), Buf()]
            npo = 0

            def d_loads(t):
                k.dma("sp", ym[t % 2][:], T.ymix[t * 128:(t + 1) * 128, :], w=[Bym[t % 2]])
                k.dma("sp", hb[t % 2][:], src_h[t * 128:(t + 1) * 128, :], r=[Bh[t]], w=[Bhb[t % 2]])

            d_loads(0)
            for t in range(NT):
                if t + 1 < NT:
                    d_loads(t + 1)
                ntmp = ntmps[t % 2]
                pT, BpT = ntmp["pT"], ntmp["BpT"]
                ymT, BymT, xTt, BxTt = ymTs[t % 2], BymTs[t % 2], xTts[t % 2], BxTts[t % 2]
                ymj, Bymj = ym[t % 2], Bym[t % 2]
                hbj, Bhbj = hb[t % 2], Bhb[t % 2]
                h1j, Bh1j = h1[t % 2], Bh1[t % 2]
                for kc in range(8):
                    k.op("pe", lambda: nc.tensor.transpose(out=pT[:, kc * 128:(kc + 1) * 128], in_=ymj[:, kc * 128:(kc + 1) * 128],
                                                           identity=C.identb[:]), r=[Bymj, C.B], w=[BpT])
                k.op("act", lambda: nc.scalar.copy(out=ymT[:], in_=pT[:].rearrange("p (c t) -> p c t", c=8)), r=[BpT], w=[BymT])
                for cg in range(2):
                    p_, Bp_ = po[npo % 2], Bpo[npo % 2]
                    npo += 1
                    for kc in range(8):
                        k.op("pe", lambda: nc.tensor.matmul(out=p_[:], lhsT=ymT[:, kc, :], rhs=Wo[:, kc, cg * 512:(cg + 1) * 512],
                                                            start=(kc == 0), stop=(kc == 7)), r=[BymT, BWo], w=[Bp_])
                    k.op("dve", lambda: V.tensor_tensor(out=h1j[:, cg * 512:(cg + 1) * 512], in0=p_[:],
                                                        in1=hbj[:, cg * 512:(cg + 1) * 512], op=ALU.add), r=[Bp_, Bhbj], w=[Bh1j])
                k.dma("sp", T.h[t * 128:(t + 1) * 128, :], h1j[:], r=[Bh1j], w=[Bh[t]])
                if "h_mix" in dbg and layer == 0:
                    k.dma("sp", dbg["h_mix"][t * 128:(t + 1) * 128, :], h1j[:], r=[Bh1j])
                norm_tile(env, s2, h1j[:], Bh1j, gmoe, Bg, xTt, BxTt, 0, ntmp)
                k.dma("sp", T.Xn[t * 128:(t + 1) * 128, :], ntmp["a"][:], r=[ntmp["Ba"]])
                RG = 8
                tj = t % RG
                pr, Bpr = prs[(t // RG) % 2], Bprs[(t // RG) % 2]
                for kc in range(8):
                    k.op("pe", lambda: nc.tensor.matmul(out=pr[:, tj, :], lhsT=xTt[:, kc, :], rhs=wr[:, kc, :],
                                                        start=(kc == 0), stop=(kc == 7)), r=[BxTt, Bg], w=[Bpr])
                if tj != RG - 1:
                    continue
                ta = t - (RG - 1)
                L, rt, Brt = Ls[(t // RG) % 2], rts[(t // RG) % 2], Brts[(t // RG) % 2]
                R = [Brt]
                f3 = lambda off, n: rt[:, off:off + RG * n].rearrange("p (a b) -> p a b", a=RG)
                f2 = lambda off: rt[:, off:off + RG]
                bc = lambda ap2, n: ap2.unsqueeze(2).broadcast_to([128, RG, n])
                gmax, gsum, pgt, m1, m2, dm, p2, t1 = (f2(i * RG) for i in range(8))
                o = 8 * RG
                ge, oh, esel, eq1, e2, eq2, tm8 = f3(o, 4), f3(o + 4 * RG, 4), f3(o + 8 * RG, 8), f3(o + 16 * RG, 8), f3(o + 24 * RG, 8), f3(o + 32 * RG, 8), f3(o + 40 * RG, 8)
                w1, w2 = W12[:, ta:t + 1, 0], W12[:, ta:t + 1, 1]
                k.op("dve", lambda: V.tensor_tensor(out=L[:], in0=pr[:], in1=brt[:].unsqueeze(1).broadcast_to([128, RG, 36]), op=ALU.add), r=[Bpr, Bg], w=R)
                k.op("dve", lambda: V.tensor_reduce(out=gmax, in_=L[:, :, 0:4], axis=AX.X, op=ALU.max), r=R, w=R)
                k.op("dve", lambda: V.tensor_tensor(out=ge, in0=L[:, :, 0:4], in1=bc(gmax, 4), op=ALU.subtract), r=R, w=R)
                k.op("act", lambda: nc.scalar.activation(out=ge, in_=ge, func=AF.Exp), r=R, w=R)
                k.op("dve", lambda: V.tensor_reduce(out=gsum, in_=ge, axis=AX.X, op=ALU.add), r=R, w=R)
                k.op("dve", lambda: V.reciprocal(out=pgt, in_=gsum), r=R, w=R)
                k.op("dve", lambda: V.tensor_tensor(out=oh, in0=L[:, :, 0:4], in1=bc(gmax, 4), op=ALU.is_equal), r=R, w=R)
                k.op("dve", lambda: V.tensor_tensor(out=esel, in0=L[:, :, 4:12], in1=oh[:, :, 0:1].broadcast_to([128, RG, 8]), op=ALU.mult), r=R, w=R)
                for gi in range(1, 4):
                    k.op("dve", lambda: V.tensor_tensor(out=tm8, in0=L[:, :, 4 + 8 * gi:12 + 8 * gi], in1=oh[:, :, gi:gi + 1].broadcast_to([128, RG, 8]),
                                                        op=ALU.mult), r=R, w=R)
                    k.op("dve", lambda: V.tensor_tensor(out=esel, in0=esel, in1=tm8, op=ALU.add), r=R, w=R)
                k.op("dve", lambda: V.tensor_reduce(out=m1, in_=esel, axis=AX.X, op=ALU.max), r=R, w=R)
                k.op("dve", lambda: V.tensor_tensor(out=eq1, in0=esel, in1=bc(m1, 8), op=ALU.is_equal), r=R, w=R)
                k.op("dve", lambda: V.scalar_tensor_tensor(out=e2, in0=eq1, scalar=NEG, in1=esel, op0=ALU.mult, op1=ALU.add), r=R, w=R)
                k.op("dve", lambda: V.tensor_reduce(out=m2, in_=e2, axis=AX.X, op=ALU.max), r=R, w=R)
                k.op("dve", lambda: V.tensor_tensor(out=eq2, in0=e2, in1=bc(m2, 8), op=ALU.is_equal), r=R, w=R)
                k.op("dve", lambda: V.tensor_tensor(out=dm, in0=m2, in1=m1, op=ALU.subtract), r=R, w=R)
                k.op("act", lambda: nc.scalar.activation(out=p2, in_=dm, func=AF.Exp), r=R, w=R)
                k.op("dve", lambda: V.tensor_scalar(out=t1, in0=p2, scalar1=1.0, scalar2=None, op0=ALU.add), r=R, w=R)
                k.op("dve", lambda: V.reciprocal(out=t1, in_=t1), r=R, w=R)
                k.op("dve", lambda: V.tensor_tensor(out=w1, in0=t1, in1=pgt, op=ALU.mult), r=R, w=R + [BOH])
                k.op("dve", lambda: V.tensor_tensor(out=w2, in0=w1, in1=p2, op=ALU.mult), r=R + [BOH], w=[BOH])
                for gi in range(4):
                    k.op("dve", lambda: V.tensor_tensor(out=OH1[:, ta:t + 1, 8 * gi:8 * gi + 8], in0=eq1, in1=oh[:, :, gi:gi + 1].broadcast_to([128, RG, 8]),
                                                        op=ALU.mult), r=R, w=[BOH])
                    k.op("dve", lambda: V.tensor_tensor(out=OH2[:, ta:t + 1, 8 * gi:8 * gi + 8], in0=eq2, in1=oh[:, :, gi:gi + 1].broadcast_to([128, RG, 8]),
                                                        op=ALU.mult), r=R, w=[BOH])
            k.barrier()
        if stop_after == ("D", layer):
            return
        with ExitStack() as s2:
            cntp = ps(s2, "T_cntp", [128, 32], F32)
            pR = [ps(s2, f"T_pR{i}", [128, 32], F32) for i in range(2)]
            pC = [ps(s2, f"T_pC{i}", [128, 32], F32) for i in range(2)]
            BpR, BpC = [Buf(), Buf()], [Buf(), Buf()]
            Bc = Buf()
            ustr = sb(s2, "T_ustr", [128, 128], F32)
            k.op("dve", lambda: V.tensor_tensor(out=ustr[:], in0=C.ufw[:], in1=C.identf[:], op=ALU.subtract), r=[C.B], w=[Bc])
            for t in range(NT):
                k.op("pe", lambda: nc.tensor.matmul(out=cntp[:], lhsT=C.ones[:], rhs=OH1[:, t, :], start=(t == 0), stop=False), r=[BOH, C.B], w=[Bc])
                k.op("pe", lambda: nc.tensor.matmul(out=cntp[:], lhsT=C.ones[:], rhs=OH2[:, t, :], start=False, stop=(t == NT - 1)), r=[BOH, C.B], w=[Bc])
            cnt = sb(s2, "T_cnt", [128, 32], F32)
            cmp3 = sb(s2, "T_cmp3", [128, 32, 32], F32)
            nblk = sb(s2, "T_nblk", [128, 32], F32)
            padded = sb(s2, "T_padded", [128, 32], F32)
            zer = sb(s2, "T_zer", [128, 32], F32)
            pend = sb(s2, "T_pend", [128, 32], F32)
            brun = sb(s2, "T_brun", [128, 32], F32)
            eb3 = sb(s2, "T_eb3", [128, NB, 32], F32)
            eb = sb(s2, "T_eb", [128, NB], F32)
            igf = sb(s2, "T_igf", [128, NB], F32)
            df = sb(s2, "T_df", [128, NT, 2], F32)
            dmt = sb(s2, "T_dmt", [128, 32], F32)
            tt = sb(s2, "T_tt", [128, 32], F32)
            R = [Bc]
            k.op("dve", lambda: V.tensor_copy(out=cnt[:], in_=cntp[:]), r=R, w=R)
            k.op("dve", lambda: V.memset(zer[:], 0.0), w=R)
            k.op("dve", lambda: V.tensor_tensor(out=cmp3[:], in0=cnt[:].unsqueeze(2).broadcast_to([128, 32, 32]),
                                                in1=C.jB[:].unsqueeze(1).broadcast_to([128, 32, 32]), op=ALU.is_gt), r=R + [C.B], w=R)
            k.op("dve", lambda: V.tensor_reduce(out=nblk[:], in_=cmp3[:], axis=AX.X, op=ALU.add), r=R, w=R)
            k.op("dve", lambda: V.tensor_scalar(out=padded[:], in0=nblk[:], scalar1=float(BS), scalar2=None, op0=ALU.mult), r=R, w=R)
            k.op("dve", lambda: V.tensor_tensor_scan(out=pend[:], data0=padded[:], data1=zer[:], initial=0.0, op0=ALU.add, op1=ALU.add), r=R, w=R)
            k.op("dve", lambda: V.tensor_tensor(out=brun[:], in0=pend[:], in1=padded[:], op=ALU.subtract), r=R, w=R)
            k.op("dve", lambda: V.tensor_tensor(out=eb3[:], in0=pend[:].unsqueeze(1).broadcast_to([128, NB, 32]),
                                                in1=C.bst[:].unsqueeze(2).broadcast_to([128, NB, 32]), op=ALU.is_le), r=R + [C.B], w=R)
            k.op("dve", lambda: V.tensor_reduce(out=eb[:], in_=eb3[:], axis=AX.X, op=ALU.add), r=R, w=R)
            k.op("dve", lambda: V.tensor_scalar(out=eb[:], in0=eb[:], scalar1=31.0, scalar2=None, op0=ALU.min), r=R, w=R)
            k.op("dve", lambda: V.tensor_scalar(out=igf[:], in0=eb[:], scalar1=128.0, scalar2=C.base8[:, 0:1], op0=ALU.mult, op1=ALU.add), r=R + [C.B], w=R)
            k.op("dve", lambda: V.tensor_copy(out=IG[:], in_=igf[:]), r=R, w=R)
            for t in range(NT):
                pr_, Bpr_ = pR[t % 2], BpR[t % 2]
                pc_, Bpc_ = pC[t % 2], BpC[t % 2]
                k.op("pe", lambda: nc.tensor.matmul(out=pr_[:], lhsT=ustr[:], rhs=OH1[:, t, :], start=True, stop=False), r=[BOH, Bc], w=[Bpr_])
                k.op("pe", lambda: nc.tensor.matmul(out=pr_[:], lhsT=ustr[:], rhs=OH2[:, t, :], start=False, stop=True), r=[BOH, Bc], w=[Bpr_])
                k.op("pe", lambda: nc.tensor.matmul(out=pc_[:], lhsT=C.ones[:], rhs=OH1[:, t, :], start=True, stop=False), r=[BOH, C.B], w=[Bpc_])
                k.op("pe", lambda: nc.tensor.matmul(out=pc_[:], lhsT=C.ones[:], rhs=OH2[:, t, :], start=False, stop=True), r=[BOH, C.B], w=[Bpc_])
                k.op("dve", lambda: V.tensor_tensor(out=dmt[:], in0=pr_[:], in1=brun[:], op=ALU.add), r=R + [Bpr_], w=R)
                k.op("dve", lambda: V.tensor_tensor(out=tt[:], in0=dmt[:], in1=OH1[:, t, :], op=ALU.mult), r=R + [BOH], w=R)
                k.op("dve", lambda: V.tensor_reduce(out=df[:, t, 0:1], in_=tt[:], axis=AX.X, op=ALU.add), r=R, w=R)
                k.op("dve", lambda: V.tensor_tensor(out=tt[:], in0=dmt[:], in1=OH2[:, t, :], op=ALU.mult), r=R + [BOH], w=R)
                k.op("dve", lambda: V.tensor_reduce(out=df[:, t, 1:2], in_=tt[:], axis=AX.X, op=ALU.add), r=R, w=R)
                k.op("dve", lambda: V.tensor_tensor(out=brun[:], in0=brun[:], in1=pc_[:], op=ALU.add), r=R + [Bpc_], w=R)
            k.op("dve", lambda: V.tensor_copy(out=DI[:], in_=df[:]), r=R, w=R)
            if "dest" in dbg and layer == 0:
                k.dma("sp", dbg["dest"], df[:], r=R)
                k.dma("sp", dbg["eb"], eb[:], r=R)
            k.barrier()
        if stop_after == ("R", layer):
            return
        with ExitStack() as s2:
            xs = [sb(s2, f"T_xs{i}", [128, D], BF16) for i in range(3)]
            Bxs = [Buf() for _ in range(3)]
            for t in range(NT):
                x_, Bx_ = xs[t % 3], Bxs[t % 3]
                k.dma("sp", x_[:], T.Xn[t * 128:(t + 1) * 128, :], w=[Bx_])
                for s_ in range(2):
                    k.idma(T.Xs, bass.IndirectOffsetOnAxis(ap=DI[:, t, s_:s_ + 1], axis=0), x_[:], None, r=[Bx_], w=[], bc=NB * BS - 1)
            k.barrier()
        if stop_after == ("S", layer):
            return
        with ExitStack() as s2:
            Wsl = [sb(s2, f"T_Wsl{i}", [128, 12288], BF16) for i in range(2)]
            BW = [Buf(), Buf()]
            xb = [sb(s2, f"T_xb{i}", [128, 4, D], BF16) for i in range(2)]
            xT = [sb(s2, f"T_xT{i}", [128, 8, 512], BF16) for i in range(2)]
            Bxb, BxT = [Buf(), Buf()], [Buf(), Buf()]
            hT = [sb(s2, f"T_hT{i}", [128, 4, 512], BF16) for i in range(2)]
            BhT = [Buf(), Buf()]
            sil = [sb(s2, f"T_sil{i}", [128, 512], F32) for i in range(2)]
            Bsil = [Buf(), Buf()]
            yo = [sb(s2, f"T_yo{i}", [128, D], BF16) for i in range(3)]
            Byo = [Buf() for _ in range(3)]
            pTs = [ps(s2, f"T_EpT{i}", [128, D], BF16) for i in range(2)]
            BpTs = [Buf(), Buf()]
            pg = [ps(s2, f"T_pg{i}", [128, 512], F32) for i in range(2)]
            pu = [ps(s2, f"T_pu{i}", [128, 512], F32) for i in range(2)]
            po = [ps(s2, f"T_Epo{i}", [128, 512], F32) for i in range(2)]
            Bpg, Bpu, Bpo = [Buf(), Buf()], [Buf(), Buf()], [Buf(), Buf()]
            ngu = 0
            npo = 0
            nyo = 0
            def e_loads(b):
                sl = b % 2
                ioff = bass.IndirectOffsetOnAxis(ap=IG[:, b:b + 1], axis=0)
                k.idma(Wsl[sl][:, 0:4096], None, weg, ioff, r=[], w=[BW[sl]])
                k.idma(Wsl[sl][:, 4096:8192], None, weu, ioff, r=[], w=[BW[sl]])
                k.idma(Wsl[sl][:, 8192:12288], None, wed, ioff, r=[], w=[BW[sl]])
                k.dma("sp", xb[sl][:], T.Xs[b * BS:(b + 1) * BS, :].rearrange("(a p) n -> p a n", p=128), w=[Bxb[sl]])

            e_loads(0)
            for b in range(NB):
                sl = b % 2
                if b + 1 < NB:
                    e_loads(b + 1)
                Wt, BWt = Wsl[sl], BW[sl]
                Wg = Wt[:, 0:4096].rearrange("p (c n) -> p c n", c=8)
                Wu = Wt[:, 4096:8192].rearrange("p (c n) -> p c n", c=8)
                Wd = Wt[:, 8192:12288].rearrange("p (c n) -> p c n", c=4)
                for a in range(4):
                    pT, BpT = pTs[a % 2], BpTs[a % 2]
                    for kc in range(8):
                        k.op("pe", lambda: nc.tensor.transpose(out=pT[:, kc * 128:(kc + 1) * 128], in_=xb[sl][:, a, kc * 128:(kc + 1) * 128],
                                                               identity=C.identb[:]), r=[Bxb[sl], C.B], w=[BpT])
                    cp = (lambda: nc.scalar.copy(out=xT[sl][:, :, a * 128:(a + 1) * 128], in_=pT[:].rearrange("p (c t) -> p c t", c=8))) if a % 2 == 0 else \
                         (lambda: V.tensor_copy(out=xT[sl][:, :, a * 128:(a + 1) * 128], in_=pT[:].rearrange("p (c t) -> p c t", c=8)))
                    k.op("act" if a % 2 == 0 else "dve", cp, r=[BpT], w=[BxT[sl]])
                hT_, BhT_ = hT[sl], BhT[sl]
                for m in range(4):
                    pg_, Bpg_ = pg[ngu % 2], Bpg[ngu % 2]
                    pu_, Bpu_ = pu[ngu % 2], Bpu[ngu % 2]
                    sl_, Bsl_ = sil[ngu % 2], Bsil[ngu % 2]
                    ngu += 1
                    for kc in range(8):
                        k.op("pe", lambda: nc.tensor.matmul(out=pg_[:], lhsT=Wg[:, kc, m * 128:(m + 1) * 128], rhs=xT[sl][:, kc, :],
                                                            start=(kc == 0), stop=(kc == 7)), r=[BWt, BxT[sl]], w=[Bpg_])
                    for kc in range(8):
                        k.op("pe", lambda: nc.tensor.matmul(out=pu_[:], lhsT=Wu[:, kc, m * 128:(m + 1) * 128], rhs=xT[sl][:, kc, :],
                                                            start=(kc == 0), stop=(kc == 7)), r=[BWt, BxT[sl]], w=[Bpu_])
                    k.op("act", lambda: nc.scalar.activation(out=sl_[:], in_=pg_[:], func=AF.Silu), r=[Bpg_], w=[Bsl_])
                    k.op("dve", lambda: V.tensor_tensor(out=hT_[:, m, :], in0=sl_[:], in1=pu_[:], op=ALU.mult), r=[Bsl_, Bpu_], w=[BhT_])
                for jj in range(4):
                    yo_, Byo_ = yo[nyo % 3], Byo[nyo % 3]
                    nyo += 1
                    for cg in range(2):
                        p_, Bp_ = po[npo % 2], Bpo[npo % 2]
                        npo += 1
                        for m in range(4):
                            k.op("pe", lambda: nc.tensor.matmul(out=p_[:], lhsT=hT_[:, m, jj * 128:(jj + 1) * 128],
                                                                rhs=Wd[:, m, cg * 512:(cg + 1) * 512], start=(m == 0), stop=(m == 3)),
                                 r=[BhT_, BWt], w=[Bp_])
                        if cg == 0:
                            k.op("act", lambda: nc.scalar.copy(out=yo_[:, 0:512], in_=p_[:]), r=[Bp_], w=[Byo_])
                        else:
                            k.op("dve", lambda: V.tensor_copy(out=yo_[:, 512:1024], in_=p_[:]), r=[Bp_], w=[Byo_])
                    k.dma("sp", T.Ys[b * BS + jj * 128:b * BS + (jj + 1) * 128, :], yo_[:], r=[Byo_])
            k.barrier()
        if stop_after == ("E", layer):
            return
        with ExitStack() as s2:
            Wpg = sb(s2, "T_Wpg", [128, 8, D], BF16)
            Wple = sb(s2, "T_Wple", [128, 2, D], BF16)
            BWp = Buf()
            k.dma("pool", Wpg[:], I.w_pg[layer].rearrange("(c p) n -> p c n", p=128), w=[BWp])
            k.dma("pool", Wple[:], I.w_ple[layer].rearrange("(c p) n -> p c n", p=128), w=[BWp])
            gple = sb(s2, "T_gple", [128, D], F32)
            gfin = sb(s2, "T_gfin", [128, D], F32)
            Bg = Buf()
            k.dma("sp", gple[:], I.g_ple[layer:layer + 1, :].broadcast_to([128, D]), w=[Bg])
            k.dma("sp", gfin[:], I.g_final[0:1, :].broadcast_to([128, D]), w=[Bg])
            ntmps = [make_norm_tmp(env, s2, f"F{i}") for i in range(2)]
            pg = [ps(s2, f"T_Fpg{i}", [128, 512], F32) for i in range(2)]
            pu = [ps(s2, f"T_Fpu{i}", [128, 512], F32) for i in range(2)]
            Bpg, Bpu = [Buf(), Buf()], [Buf(), Buf()]
            y1 = [sb(s2, f"T_y1{i}", [128, D], BF16) for i in range(3)]
            y2 = [sb(s2, f"T_y2{i}", [128, D], BF16) for i in range(3)]
            hb = [sb(s2, f"T_Fhb{i}", [128, D], F32) for i in range(3)]
            By1, By2, Bhb = ([Buf() for _ in range(3)] for _ in range(3))
            aTps = [sb(s2, f"T_aTp{i}", [128, 8, 128], BF16) for i in range(2)]
            BaTps = [Buf(), Buf()]
            sigs = [sb(s2, f"T_sig{i}", [128, D], F32) for i in range(2)]
            Bsigs = [Buf(), Buf()]
            h2 = [sb(s2, f"T_h2{i}", [128, D], F32) for i in range(2)]
            Bh2 = [Buf(), Buf()]
            pins = [sb(s2, f"T_pin{i}", [128, 256], F32) for i in range(3)]
            pbfs = [sb(s2, f"T_pbf{i}", [128, 256], BF16) for i in range(2)]
            ppTs = [sb(s2, f"T_ppT{i}", [128, 2, 128], BF16) for i in range(2)]
            Bpins, Bpbfs, BppTs = [Buf(), Buf(), Buf()], [Buf(), Buf()], [Buf(), Buf()]
            fsss = [sb(s2, f"T_fss{i}", [128, 2], F32) for i in range(2)]
            Bfsss = [Buf(), Buf()]
            def f_loads(t):
                j2 = t % 3
                k.idma(y1[j2][:], None, T.Ys, bass.IndirectOffsetOnAxis(ap=DI[:, t, 0:1], axis=0), r=[], w=[By1[j2]])
                k.idma(y2[j2][:], None, T.Ys, bass.IndirectOffsetOnAxis(ap=DI[:, t, 1:2], axis=0), r=[], w=[By2[j2]])
                k.dma("sp", hb[j2][:], T.h[t * 128:(t + 1) * 128, :], r=[Bh[t]], w=[Bhb[j2]])
                k.dma("sp", pins[j2][:], I.p[layer, t * 128:(t + 1) * 128, :], w=[Bpins[j2]])

            def f_stage1(t):
                i2 = t % 2
                i3 = t % 3
                ntmp = ntmps[i2]
                pT, BpT = ntmp["pT"], ntmp["BpT"]
                k.op("dve", lambda: V.scalar_tensor_tensor(out=hb[i3][:], in0=y1[i3][:], scalar=W12[:, t, 0:1], in1=hb[i3][:],
                                                            op0=ALU.mult, op1=ALU.add), r=[By1[i3], Bhb[i3], BOH], w=[Bhb[i3]])
                k.op("dve", lambda: V.scalar_tensor_tensor(out=hb[i3][:], in0=y2[i3][:], scalar=W12[:, t, 1:2], in1=hb[i3][:],
                                                            op0=ALU.mult, op1=ALU.add), r=[By2[i3], Bhb[i3], BOH], w=[Bhb[i3]])
                if "h_moe" in dbg and layer == 0:
                    k.dma("sp", dbg["h_moe"][t * 128:(t + 1) * 128, :], hb[i3][:], r=[Bhb[i3]])
                norm_pre(env, hb[i3][:], Bhb[i3], gple, Bg, ntmp)
                k.op("dve", lambda: V.tensor_copy(out=pbfs[i2][:], in_=pins[i3][:]), r=[Bpins[i3]], w=[Bpbfs[i2]])
                norm_post(env, aTps[i2], BaTps[i2], 0, ntmp)
                for kc in range(2):
                    k.op("pe", lambda: nc.tensor.transpose(out=pT[:, kc * 128:(kc + 1) * 128], in_=pbfs[i2][:, kc * 128:(kc + 1) * 128],
                                                           identity=C.identb[:]), r=[Bpbfs[i2], C.B], w=[BpT])
                k.op("act", lambda: nc.scalar.copy(out=ppTs[i2][:], in_=pT[:, 0:256].rearrange("p (c t) -> p c t", c=2)), r=[BpT], w=[BppTs[i2]])

            def f_stage2(t):
                i2 = t % 2
                aTp, BaTp, sig, Bsig = aTps[i2], BaTps[i2], sigs[i2], Bsigs[i2]
                ppT, BppT = ppTs[i2], BppTs[i2]
                fss, Bfss = fsss[i2], Bfsss[i2]
                h2_, Bh2_ = h2[i2], Bh2[i2]
                for cg in range(2):
                    cs_ = slice(cg * 512, (cg + 1) * 512)
                    pg_, Bpg_ = pg[cg], Bpg[cg]
                    pu_, Bpu_ = pu[cg], Bpu[cg]
                    for kc in range(8):
                        k.op("pe", lambda: nc.tensor.matmul(out=pg_[:], lhsT=aTp[:, kc, :], rhs=Wpg[:, kc, cs_], start=(kc == 0), stop=(kc == 7)),
                             r=[BaTp, BWp], w=[Bpg_])
                    k.op("act", lambda: nc.scalar.activation(out=sig[:, cs_], in_=pg_[:], func=AF.Sigmoid), r=[Bpg_], w=[Bsig])
                    for kc in range(2):
                        k.op("pe", lambda: nc.tensor.matmul(out=pu_[:], lhsT=ppT[:, kc, :], rhs=Wple[:, kc, cs_], start=(kc == 0), stop=(kc == 1)),
                             r=[BppT, BWp], w=[Bpu_])
                    k.op("dve", lambda: V.tensor_tensor(out=h2_[:, cs_], in0=pu_[:], in1=sig[:, cs_], op=ALU.mult), r=[Bpu_, Bsig], w=[Bh2_])
                k.op("dve", lambda: V.tensor_tensor(out=h2_[:], in0=h2_[:], in1=hb[t % 3][:], op=ALU.add), r=[Bh2_, Bhb[t % 3]], w=[Bh2_])
                if not last:
                    k.dma("sp", T.h[t * 128:(t + 1) * 128, :], h2_[:], r=[Bh2_], w=[Bh[t]])
                    if "h_ple" in dbg and layer == 0:
                        k.dma("sp", dbg["h_ple"][t * 128:(t + 1) * 128, :], h2_[:], r=[Bh2_])
                else:
                    k.op("act", lambda: nc.scalar.activation(out=sig[:], in_=h2_[:], func=AF.Square, accum_out=fss[:, 0:1]), r=[Bh2_], w=[Bsig, Bfss])
                    k.op("act", lambda: nc.scalar.activation(out=fss[:, 1:2], in_=fss[:, 0:1], func=AF.Ln, bias=EPS, scale=1.0 / D), r=[Bfss], w=[Bfss])
                    k.op("act", lambda: nc.scalar.activation(out=fss[:, 1:2], in_=fss[:, 1:2], func=AF.Exp, scale=-0.5), r=[Bfss], w=[Bfss])
                    k.op("dve", lambda: V.scalar_tensor_tensor(out=h2_[:], in0=h2_[:], scalar=fss[:, 1:2], in1=gfin[:], op0=ALU.mult, op1=ALU.mult),
                         r=[Bh2_, Bfss, Bg], w=[Bh2_])
                    k.dma("sp", out[t * 128:(t + 1) * 128, :], h2_[:], r=[Bh2_])

            f_loads(0)
            f_loads(1)
            f_stage1(0)
            for t in range(NT):
                if t + 2 < NT:
                    f_loads(t + 2)
                if t + 1 < NT:
                    f_stage1(t + 1)
                f_stage2(t)
            k.barrier()


def _prep_inputs(inputs):
    f = lambda a: np.ascontiguousarray(np.asarray(a, dtype=np.float32))
    shared = {}
    for name in ("w_in", "b_gate", "conv_w", "conv_b", "g_na", "g_ml", "w_out", "g_mix", "g_moe",
                 "g_ple", "w_ple", "w_ple_gate"):
        shared[name] = f(inputs[name])
    for name, kc in (("w_exp_gate", 8), ("w_exp_up", 8), ("w_exp_down", 4)):
        w = np.asarray(inputs[name], dtype=np.float32)
        n = w.shape[-1]
        w = w.reshape(DEPTH, 32, kc, 128, n).transpose(0, 1, 3, 2, 4)
        shared[name] = np.ascontiguousarray(w).reshape(DEPTH * 32 * 128, kc * n)
    shared["w_rt"] = f(np.concatenate([inputs["w_route_group"], inputs["w_route_expert"]], axis=-1))
    shared["b_rt"] = f(np.concatenate([inputs["b_route_group"], inputs["b_route_expert"]], axis=-1))
    shared["g_final"] = f(inputs["g_final"]).reshape(1, D)
    rpb = f(inputs["rpb"])
    shared["natab"] = np.stack([_na_tables(rpb[l]).reshape(5, 128, 8 * 5 * 128) for l in range(DEPTH)])
    shared.update(_consts())
    x = f(inputs["x"])
    p = f(inputs["p"])
    in_maps = []
    for b in range(8):
        m = dict(shared)
        m["x"] = x[b]
        m["p"] = np.ascontiguousarray(p[:, b])
        in_maps.append(m)
    return in_maps


def kernel(**inputs):
    in_maps = _prep_inputs(inputs)
    nc = build_program()
    res = run_bass_kernel_spmd(nc, in_maps, core_ids=list(range(8)))
    return np.stack([np.asarray(r["out"], dtype=np.float32) for r in res.results], axis=0)
```

```python
import numpy as np
from contextlib import ExitStack
import concourse.bass as bass
import concourse.mybir as mybir
from concourse.bass_utils import run_bass_kernel_spmd

F32 = mybir.dt.float32
BF16 = mybir.dt.bfloat16
AF = mybir.ActivationFunctionType
ALU = mybir.AluOpType
AX = mybir.AxisListType

S = 8192
D = 1024
NT = 64
DEPTH = 2
D_IN = 3600
EPS = 1e-6
NEG = -1e30


class Buf:
    __slots__ = ("last_w", "readers")

    def __init__(self):
        self.last_w = None
        self.readers = []


class KB:
    COMPUTE = ("pe", "dve", "act", "pool")

    def __init__(self, nc, stack, n_dma_sems=(("sp", 20), ("pool", 24), ("bg", 48))):
        self.nc = nc
        self.e = dict(pe=nc.tensor, dve=nc.vector, act=nc.scalar, pool=nc.gpsimd, sp=nc.sync)
        self.csem = {k: stack.enter_context(nc.semaphore(f"c_{k}")) for k in self.COMPUTE}
        self.cnt = {k: 0 for k in self.COMPUTE}
        self.seen = {k: {} for k in self.e}
        self.seen["bg"] = self.seen["pool"]
        self.dsem = {}
        self.dpos = {}
        for q, n in n_dma_sems:
            self.dsem[q] = [[stack.enter_context(nc.semaphore(f"d_{q}{i}")), 0, None] for i in range(n)]
            self.dpos[q] = 0

    def _wait(self, eng, tok):
        if tok is None:
            return
        sem, val = tok
        key = id(sem)
        if self.seen[eng].get(key, 0) >= val:
            return
        self.e[eng].wait_ge(sem, val)
        self.seen[eng][key] = val

    def _deps(self, eng, r, w):
        own = self.csem.get(eng)
        best = {}

        def add(t):
            k = id(t[0])
            if k not in best or best[k][1] < t[1]:
                best[k] = t
        for b in r:
            if b.last_w is not None:
                add(b.last_w)
        for b in w:
            if b.last_w is not None and b.last_w[0] is not own:
                add(b.last_w)
            for t in b.readers:
                if t[0] is not own:
                    add(t)
        for t in best.values():
            self._wait(eng, t)

    def _commit(self, tok, r, w):
        for b in r:
            b.readers.append(tok)
            if len(b.readers) > 48:
                best = {}
                for t in b.readers:
                    k = id(t[0])
                    if k not in best or best[k][1] < t[1]:
                        best[k] = t
                b.readers = list(best.values())
        for b in w:
            b.last_w = tok
            b.readers = []

    def op(self, eng, fn, r=(), w=()):
        self._deps(eng, r, w)
        ins = fn()
        self.cnt[eng] += 1
        ins.then_inc(self.csem[eng], 1)
        tok = (self.csem[eng], self.cnt[eng])
        self._commit(tok, r, w)
        return tok

    def dma(self, q, out, in_, r=(), w=(), ring=None, **kw):
        self._deps(q, r, w)
        rq = ring or q
        ring = self.dsem[rq]
        slot = ring[self.dpos[rq] % len(ring)]
        self.dpos[rq] += 1
        if slot[2] is not None:
            self._wait(q, slot[2])
        ins = self.e[q].dma_start(out=out, in_=in_, **kw)
        slot[1] += 16
        ins.then_inc(slot[0], 16)
        tok = (slot[0], slot[1])
        slot[2] = tok
        self._commit(tok, r, w)
        return tok

    def idma(self, out, out_off, in_, in_off, r=(), w=(), bc=None):
        q = "pool"
        self._deps(q, r, w)
        ring = self.dsem[q]
        slot = ring[self.dpos[q] % len(ring)]
        self.dpos[q] += 1
        if slot[2] is not None:
            self._wait(q, slot[2])
        ins = self.nc.gpsimd.indirect_dma_start(out=out, out_offset=out_off, in_=in_, in_offset=in_off)
        slot[1] += 16
        ins.then_inc(slot[0], 16)
        tok = (slot[0], slot[1])
        slot[2] = tok
        self._commit(tok, r, w)
        return tok

    def barrier(self):
        toks = [(self.csem[k], self.cnt[k]) for k in self.COMPUTE if self.cnt[k] > 0]
        for q in self.dsem:
            for s in self.dsem[q]:
                if s[2] is not None:
                    toks.append(s[2])
        for eng in self.e:
            for t in toks:
                self._wait(eng, t)


class Ctx:
    pass


def _consts():
    ii = np.arange(128)
    c = {}
    c["ident"] = np.eye(128, dtype=np.float32)
    c["antiid"] = np.eye(128, dtype=np.float32)[::-1].copy()
    c["ufw"] = (ii[:, None] <= ii[None, :]).astype(np.float32)
    c["ubw"] = (ii[:, None] >= ii[None, :]).astype(np.float32)
    c["ones"] = np.ones((128, 128), np.float32)
    c["base8"] = (np.arange(8)[None, :] * 128 + ii[:, None]).astype(np.float32)
    c["jB"] = np.broadcast_to((np.arange(32) * 512).astype(np.float32)[None, :], (128, 32)).copy()
    c["bst"] = np.broadcast_to((np.arange(64) * 512).astype(np.float32)[None, :], (128, 64)).copy()
    return c


def _na_tables(rpb):
    H = 8
    out = np.empty((5, 128, H, 5, 128), np.float32)
    kp = np.arange(128)
    ql = np.arange(128)
    for ti, u in enumerate((0, 1, 2, 62, 63)):
        t0 = min(max(u - 2, 0), 59)
        r = 2 * u + ql // 64
        c = ql % 64
        rs = np.clip(r - 4, 0, 120)
        cs = np.clip(c - 8, 0, 48)
        for j in range(5):
            kr = 2 * (t0 + j) + kp // 64
            kc = kp % 64
            inwin = ((kr[:, None] >= rs[None, :]) & (kr[:, None] < rs[None, :] + 8) &
                     (kc[:, None] >= cs[None, :]) & (kc[:, None] < cs[None, :] + 16))
            dr = np.clip(kr[:, None] - r[None, :] + 7, 0, 14)
            dc = np.clip(kc[:, None] - c[None, :], -15, 15) + 15
            b = rpb[:, dr, dc]
            b = np.where(inwin[None], b, np.float32(NEG))
            out[ti, :, :, j, :] = b.transpose(1, 0, 2)
    return out


def build_program(debug=None, stop_after=None):
    nc = bass.Bass("TRN2", target_bir_lowering=False)
    dt_in = lambda name, shape, dt=F32: nc.dram_tensor(name, list(shape), dt, kind="ExternalInput").ap()
    dt_out = lambda name, shape, dt=F32: nc.dram_tensor(name, list(shape), dt, kind="ExternalOutput").ap()
    dt_tmp = lambda name, shape, dt=F32: nc.dram_tensor(name, list(shape), dt).ap()

    I = Ctx()
    I.x = dt_in("x", [S, D])
    I.p = dt_in("p", [DEPTH, S, 256])
    I.w_in = dt_in("w_in", [DEPTH, D, D_IN])
    I.b_gate = dt_in("b_gate", [DEPTH, 16])
    I.conv_w = dt_in("conv_w", [DEPTH, 5, 1024])
    I.conv_b = dt_in("conv_b", [DEPTH, 1024])
    I.natab = dt_in("natab", [DEPTH, 5, 128, 8 * 5 * 128])
    I.g_na = dt_in("g_na", [DEPTH, 512])
    I.g_ml = dt_in("g_ml", [DEPTH, 512])
    I.w_out = dt_in("w_out", [DEPTH, D, D])
    I.g_mix = dt_in("g_mix", [DEPTH, D])
    I.g_moe = dt_in("g_moe", [DEPTH, D])
    I.w_rt = dt_in("w_rt", [DEPTH, D, 36])
    I.b_rt = dt_in("b_rt", [DEPTH, 36])
    I.w_eg = dt_in("w_exp_gate", [DEPTH * 32 * 128, 8 * 512])
    I.w_eu = dt_in("w_exp_up", [DEPTH * 32 * 128, 8 * 512])
    I.w_ed = dt_in("w_exp_down", [DEPTH * 32 * 128, 4 * D])
    I.g_ple = dt_in("g_ple", [DEPTH, D])
    I.w_ple = dt_in("w_ple", [DEPTH, 256, D])
    I.w_pg = dt_in("w_ple_gate", [DEPTH, D, D])
    I.g_final = dt_in("g_final", [1, D])
    I.ident = dt_in("ident", [128, 128])
    I.antiid = dt_in("antiid", [128, 128])
    I.ufw = dt_in("ufw", [128, 128])
    I.ubw = dt_in("ubw", [128, 128])
    I.ones = dt_in("ones", [128, 128])
    I.base8 = dt_in("base8", [128, 8])
    I.jB = dt_in("jB", [128, 32])
    I.bst = dt_in("bst", [128, 64])
    out = dt_out("out", [S, D])

    T = Ctx()
    T.h = dt_tmp("h_scr", [S, D])
    T.qnaT = dt_tmp("qnaT", [512, S], BF16)
    T.knaT = dt_tmp("knaT", [512, S], BF16)
    T.vna = dt_tmp("vna", [S, 520], BF16)
    T.qkT = dt_tmp("qkT", [1024, S], BF16)
    T.vml = dt_tmp("vml", [S, 512], BF16)
    T.sigo = dt_tmp("sigo", [S, 512], BF16)
    T.gatesP = dt_tmp("gatesP", [128, NT, 16])
    T.ymix = dt_tmp("ymix", [S, D], BF16)
    T.sc = dt_tmp("sc_small", [16, 256])
    T.Xn = dt_tmp("Xn", [S, D], BF16)
    T.Xs = dt_tmp("Xs", [64 * 512, D], BF16)
    T.Ys = dt_tmp("Ys", [64 * 512, D], BF16)
    T.Wbf = [dt_tmp(f"Wbf{m}", [32 * 128, 4096], BF16) for m in range(3)]
    dbg = {}
    if debug:
        for name, shape, dtt in debug:
            dbg[name] = dt_out("dbg_" + name, shape, dtt)

    with ExitStack() as st0:
        k = KB(nc, st0)
        uid = [0]

        def sb(stack, name, shape, dt):
            uid[0] += 1
            return stack.enter_context(nc.sbuf_tensor(f"{name}_{uid[0]}", list(shape), dt))

        def ps(stack, name, shape, dt):
            uid[0] += 1
            return stack.enter_context(nc.psum_tensor(f"{name}_{uid[0]}", list(shape), dt))

        C = Ctx()
        C.identf = sb(st0, "identf", [128, 128], F32)
        C.identb = sb(st0, "identb", [128, 128], BF16)
        C.antif = sb(st0, "antif", [128, 128], F32)
        C.ufw = sb(st0, "c_ufw", [128, 128], F32)
        C.ubw = sb(st0, "c_ubw", [128, 128], F32)
        C.ones = sb(st0, "c_ones", [128, 128], F32)
        C.base8 = sb(st0, "c_base8", [128, 8], F32)
        C.jB = sb(st0, "c_jB", [128, 32], F32)
        C.bst = sb(st0, "c_bst", [128, 64], F32)
        C.B = Buf()
        for t, src in ((C.identf, I.ident), (C.antif, I.antiid), (C.ufw, I.ufw), (C.ubw, I.ubw), (C.ones, I.ones),
                       (C.base8, I.base8), (C.jB, I.jB), (C.bst, I.bst)):
            k.dma("sp", t[:], src, w=[C.B])
        k.op("dve", lambda: nc.vector.tensor_copy(out=C.identb[:], in_=C.identf[:]), r=[C.B], w=[C.B])
        k.barrier()

        Bh = [Buf() for _ in range(NT)]
        env = dict(nc=nc, k=k, I=I, T=T, C=C, sb=sb, ps=ps, Bh=Bh, out=out, dbg=dbg)

        for layer in range(DEPTH):
            src_h = I.x if layer == 0 else T.h
            stage_inproj(env, layer, src_h)
            k.barrier()
            if stop_after == ("A", layer):
                break
            stage_na(env, layer)
            k.barrier()
            if stop_after == ("B", layer):
                break
            stage_mlstm(env, layer)
            k.barrier()
            if stop_after == ("C", layer):
                break
            stage_tail(env, layer, src_h, stop_after)
            k.barrier()
            if stop_after is not None and stop_after[1] == layer:
                break
        for name in dbg:
            src = dict(z_vna=T.vna, z_qnaT=T.qnaT, z_knaT=T.knaT, z_qkT=T.qkT, z_vml=T.vml, z_sigo=T.sigo,
                       z_gatesP=T.gatesP, ymix=T.ymix, h=T.h).get(name)
            if src is not None:
                k.dma("sp", dbg[name], src)
        k.barrier()
    return nc


def norm_pre(env, h_sb, Bh_sb, g_bc, Bg, tmp):
    nc, k = env["nc"], env["k"]
    k.op("act", lambda: nc.scalar.activation(out=tmp["sq"][:], in_=h_sb, func=AF.Square, accum_out=tmp["ss"][:]),
         r=[Bh_sb], w=[tmp["Bsq"], tmp["Bss"]])
    k.op("act", lambda: nc.scalar.activation(out=tmp["rs"][:], in_=tmp["ss"][:], func=AF.Ln, bias=EPS, scale=1.0 / D),
         r=[tmp["Bss"]], w=[tmp["Brs"]])
    k.op("act", lambda: nc.scalar.activation(out=tmp["rs"][:], in_=tmp["rs"][:], func=AF.Exp, scale=-0.5), r=[tmp["Brs"]], w=[tmp["Brs"]])
    k.op("dve", lambda: nc.vector.scalar_tensor_tensor(out=tmp["a"][:], in0=h_sb, scalar=tmp["rs"][:], in1=g_bc[:],
                                                        op0=ALU.mult, op1=ALU.mult),
         r=[Bh_sb, tmp["Brs"], Bg], w=[tmp["Ba"]])


def norm_post(env, aT, BaT, col0, tmp):
    nc, k, C = env["nc"], env["k"], env["C"]
    for kc in range(8):
        k.op("pe", lambda: nc.tensor.transpose(out=tmp["pT"][:, kc * 128:(kc + 1) * 128],
                                               in_=tmp["a"][:, kc * 128:(kc + 1) * 128], identity=C.identb[:]),
             r=[tmp["Ba"], C.B], w=[tmp["BpT"]])
    k.op("act", lambda: nc.scalar.copy(out=aT[:, :, col0:col0 + 128],
                                       in_=tmp["pT"][:].rearrange("p (c t) -> p c t", c=8)),
         r=[tmp["BpT"]], w=[BaT])


def norm_tile(env, stk_tiles, h_sb, Bh_sb, g_bc, Bg, aT, BaT, col0, tmp):
    norm_pre(env, h_sb, Bh_sb, g_bc, Bg, tmp)
    norm_post(env, aT, BaT, col0, tmp)


def make_norm_tmp(env, stk, tag):
    sb, ps = env["sb"], env["ps"]
    t = {}
    t["sq"] = sb(stk, f"nsq{tag}", [128, D], F32)
    t["ss"] = sb(stk, f"nss{tag}", [128, 1], F32)
    t["rs"] = sb(stk, f"nrs{tag}", [128, 1], F32)
    t["a"] = sb(stk, f"na{tag}", [128, D], BF16)
    t["pT"] = ps(stk, f"npT{tag}", [128, D], BF16)
    for n in ("Bsq", "Bss", "Brs", "Ba", "BpT"):
        t[n] = Buf()
    return t


def stage_inproj(env, layer, src_h):
    nc, k, I, T, C, sb, ps, Bh = (env[n] for n in ("nc", "k", "I", "T", "C", "sb", "ps", "Bh"))
    with ExitStack() as stk:
        W = sb(stk, "A_w", [128, 8, D_IN], BF16)
        BW = Buf()
        wsrc = I.w_in[layer].rearrange("(c p) n -> p c n", p=128)
        for kc in range(8):
            k.dma("pool", W[:, kc, :], wsrc[:, kc, :], w=[BW])
        gbc = sb(stk, "A_g", [128, D], F32)
        Bg = Buf()
        k.dma("sp", gbc[:], I.g_mix[layer:layer + 1, :].broadcast_to([128, D]), w=[Bg])
        bgate = sb(stk, "A_bg", [128, 16], F32)
        k.dma("sp", bgate[:], I.b_gate[layer:layer + 1, :].broadcast_to([128, 16]), w=[Bg])
        ntmps = [make_norm_tmp(env, stk, f"A{i}") for i in range(2)]
        hs = [sb(stk, f"A_h{i}", [128, D], F32) for i in range(8)]
        Bhs = [Buf() for _ in range(8)]
        aT = [sb(stk, f"A_aT{i}", [128, 8, 512], BF16) for i in range(2)]
        BaT = [Buf(), Buf()]
        pF = [ps(stk, f"A_pF{i}", [128, 512], F32) for i in range(2)]
        BpF = [Buf(), Buf()]
        pTk = [ps(stk, f"A_pT{i}", [128, 512], F32) for i in range(3)]
        BpTk = [Buf() for _ in range(3)]
        zF = [sb(stk, f"A_zF{i}", [128, 512], BF16) for i in range(4)]
        BzF = [Buf() for _ in range(4)]
        zT = [sb(stk, f"A_zT{i}", [128, 512], BF16) for i in range(4)]
        BzT = [Buf() for _ in range(4)]
        gt = [sb(stk, f"A_gt{i}", [128, 16], F32) for i in range(2)]
        Bgt = [Buf(), Buf()]
        zV = [sb(stk, f"A_zV{i}", [128, 8, 65], BF16) for i in range(2)]
        BzV = [Buf(), Buf()]
        for i in range(2):
            k.op("pool", lambda: nc.gpsimd.memset(zV[i][:], 1.0), w=[BzV[i]])
        fm = []
        for c in range(4):
            fm.append((c * 128, T.qnaT, c * 128, 0.125))
        for c in range(4):
            fm.append((512 + c * 128, T.knaT, c * 128, 1.0))
        for c in range(8):
            fm.append((1536 + c * 128, T.qkT, c * 128, 1.0))
        nF = 0
        nTk = 0
        nz = 0

        def a_loads(g):
            for j in range(4):
                t = g * 4 + j
                i8 = (g % 2) * 4 + j
                k.dma("sp", hs[i8][:], src_h[t * 128:(t + 1) * 128, :], r=[Bh[t]], w=[Bhs[i8]])

        def a_pre(g, j):
            i8 = (g % 2) * 4 + j
            norm_pre(env, hs[i8][:], Bhs[i8], gbc, Bg, ntmps[j % 2])

        def a_post(g, j):
            norm_post(env, aT[g % 2], BaT[g % 2], j * 128, ntmps[j % 2])

        a_loads(0)
        for j in range(4):
            a_pre(0, j)
            a_post(0, j)
        for g in range(NT // 4):
            a_t = aT[g % 2]
            Ba = BaT[g % 2]
            nxt = g + 1 < NT // 4
            if nxt:
                a_loads(g + 1)
            for (zc, dst, drow, scale) in fm:
                pf = pF[nF % 2]
                Bp = BpF[nF % 2]
                nF += 1
                for kc in range(8):
                    k.op("pe", lambda: nc.tensor.matmul(out=pf[:], lhsT=W[:, kc, zc:zc + 128], rhs=a_t[:, kc, :],
                                                        start=(kc == 0), stop=(kc == 7)), r=[BW, Ba], w=[Bp])
                zf = zF[nz % 4]
                Bz = BzF[nz % 4]
                nz += 1
                k.op("act", lambda: nc.scalar.activation(out=zf[:], in_=pf[:], func=AF.Copy, scale=scale), r=[Bp], w=[Bz])
                k.dma("sp", dst[drow:drow + 128, g * 512:(g + 1) * 512], zf[:], r=[Bz])
            for j in range(4):
                t = g * 4 + j
                if nxt:
                    a_pre(g + 1, j)
                for which, (zc, n) in enumerate(((1024, 512), (2560, 512), (3072, 512), (3584, 16))):
                    pt = pTk[nTk % 3]
                    Bp = BpTk[nTk % 3]
                    nTk += 1
                    for kc in range(8):
                        k.op("pe", lambda: nc.tensor.matmul(out=pt[:, 0:n], lhsT=a_t[:, kc, j * 128:(j + 1) * 128],
                                                            rhs=W[:, kc, zc:zc + n], start=(kc == 0), stop=(kc == 7)),
                             r=[BW, Ba], w=[Bp])
                    if which == 0:
                        zv, Bzv = zV[t % 2], BzV[t % 2]
                        k.op("dve", lambda: nc.vector.tensor_copy(out=zv[:, :, 0:64], in_=pt[:].rearrange("p (h d) -> p h d", d=64)),
                             r=[Bp], w=[Bzv])
                        k.dma("sp", T.vna[t * 128:(t + 1) * 128, :], zv[:].rearrange("p h e -> p (h e)"), r=[Bzv])
                    elif which < 3:
                        zt = zT[(nz) % 4]
                        Bz = BzT[(nz) % 4]
                        nz += 1
                        if which == 2:
                            k.op("act", lambda: nc.scalar.activation(out=zt[:], in_=pt[:], func=AF.Sigmoid), r=[Bp], w=[Bz])
                        else:
                            k.op("dve", lambda: nc.vector.tensor_copy(out=zt[:], in_=pt[:]), r=[Bp], w=[Bz])
                        dst = (T.vna, T.vml, T.sigo)[which]
                        k.dma("sp", dst[t * 128:(t + 1) * 128, :], zt[:], r=[Bz])
                    else:
                        g_t = gt[t % 2]
                        Bg_t = Bgt[t % 2]
                        k.op("dve", lambda: nc.vector.tensor_tensor(out=g_t[:], in0=pt[:, 0:16], in1=bgate[:], op=ALU.add),
                             r=[Bp, Bg], w=[Bg_t])
                        k.dma("sp", T.gatesP[:, t, :], g_t[:], r=[Bg_t])
                if nxt:
                    a_post(g + 1, j)


def stage_na(env, layer):
    nc, k, I, T, C, sb, ps = (env[n] for n in ("nc", "k", "I", "T", "C", "sb", "ps"))
    with ExitStack() as stk:
        tabI = sb(stk, "N_tabI", [128, 8 * 640], F32)
        tabE = [sb(stk, f"N_tabE{i}", [128, 8 * 640], F32) for i in range(2)]
        BtI, BtE = Buf(), [Buf(), Buf()]
        k.dma("sp", tabI[:], I.natab[layer, 2], w=[BtI])
        gna = sb(stk, "N_g", [128, 512], F32)
        Bg = Buf()
        k.dma("sp", gna[:], I.g_na[layer:layer + 1, :].broadcast_to([128, 512]), w=[Bg])
        NBUF = 3
        Kt = [sb(stk, f"N_K{i}", [128, 4, 640], BF16) for i in range(NBUF)]
        Qt = [sb(stk, f"N_Q{i}", [128, 4, 128], BF16) for i in range(NBUF)]
        Vt = [sb(stk, f"N_V{i}", [128, 5, 8, 65], BF16) for i in range(NBUF)]
        BK, BQ, BV = ([Buf() for _ in range(NBUF)] for _ in range(3))
        pS = [ps(stk, f"N_pS{i}", [128, 1536], F32) for i in range(2)]
        BpS = [Buf(), Buf()]
        pO = ps(stk, "N_pO", [128, 2, 512], F32)
        BpO = Buf()
        sc = [sb(stk, f"N_sc{i}", [128, 1280], F32) for i in range(2)]
        pr = [sb(stk, f"N_pr{i}", [128, 1280], BF16) for i in range(2)]
        Bsc, Bpr = [Buf(), Buf()], [Buf(), Buf()]
        rden = sb(stk, "N_rden", [128, 8], F32)
        y = sb(stk, "N_y", [128, 8, 64], F32)
        ysq = sb(stk, "N_ysq", [128, 8, 64], F32)
        ssq = sb(stk, "N_ssq", [128, 8], F32)
        yb = [sb(stk, f"N_yb{i}", [128, 512], BF16) for i in range(2)]
        Brd, By, Bysq, Bssq, Byb = Buf(), Buf(), Buf(), Buf(), [Buf(), Buf()]
        qsrc = T.qnaT.rearrange("(c p) s -> p c s", p=128)
        ksrc = T.knaT.rearrange("(c p) s -> p c s", p=128)
        tabs = {}

        def loads(u):
            t0 = min(max(u - 2, 0), 59)
            b = u % NBUF
            k.dma("sp", Kt[b][:], ksrc[:, :, t0 * 128:t0 * 128 + 640], w=[BK[b]])
            k.dma("sp", Qt[b][:], qsrc[:, :, u * 128:(u + 1) * 128], w=[BQ[b]])
            k.dma("sp", Vt[b][:].rearrange("p j h e -> p j (h e)"),
                  T.vna[t0 * 128:t0 * 128 + 640, :].rearrange("(j p) n -> p j n", p=128), w=[BV[b]])
            if u in (0, 1, 62, 63):
                ti = {0: 0, 1: 1, 62: 3, 63: 4}[u]
                k.dma("sp", tabE[u % 2][:], I.natab[layer, ti], w=[BtE[u % 2]])
                tabs[u] = (tabE[u % 2], BtE[u % 2])
            else:
                tabs[u] = (tabI, BtI)

        def scores(i):
            u, un = divmod(i, 4)
            if un == 0:
                loads(u)
            b = u % NBUF
            for hh in range(2):
                h = un * 2 + hh
                pb = (h % 2) * 64
                for j in range(5):
                    k.op("pe", lambda: nc.tensor.matmul(out=pS[i % 2][:, hh * 640 + j * 128: hh * 640 + (j + 1) * 128],
                                                        lhsT=Kt[b][pb:pb + 64, h // 2, j * 128:(j + 1) * 128],
                                                        rhs=Qt[b][pb:pb + 64, h // 2, :], start=True, stop=True),
                         r=[BK[b], BQ[b]], w=[BpS[i % 2]])

        def softmax_pv(i):
            u, un = divmod(i, 4)
            b = u % NBUF
            tab, Bt = tabs[u]
            k.op("dve", lambda: nc.vector.tensor_tensor(out=sc[i % 2][:], in0=pS[i % 2][:, 0:1280],
                                                        in1=tab[:, un * 1280:(un + 1) * 1280], op=ALU.add),
                 r=[BpS[i % 2], Bt], w=[Bsc[i % 2]])
            k.op("act", lambda: nc.scalar.activation(out=pr[i % 2][:], in_=sc[i % 2][:], func=AF.Exp), r=[Bsc[i % 2]], w=[Bpr[i % 2]])
            for hh in range(2):
                h = un * 2 + hh
                for j in range(5):
                    k.op("pe", lambda: nc.tensor.matmul(out=pO[:, h // 4, (h % 4) * 65:(h % 4) * 65 + 65],
                                                        lhsT=pr[i % 2][:, hh * 640 + j * 128: hh * 640 + (j + 1) * 128],
                                                        rhs=Vt[b][:, j, h, :], start=(j == 0), stop=(j == 4)),
                         r=[Bpr[i % 2], BV[b]], w=[BpO])

        def epilogue(u):
            o4 = pO[:, :, 0:260].rearrange("p a (h e) -> p a h e", e=65)
            k.op("dve", lambda: nc.vector.reciprocal(out=rden[:].rearrange("p (a h) -> p a h", a=2), in_=o4[:, :, :, 64]),
                 r=[BpO], w=[Brd])
            k.op("dve", lambda: nc.vector.tensor_tensor(out=y[:].rearrange("p (a h) d -> p a h d", a=2), in0=o4[:, :, :, 0:64],
                                                        in1=rden[:].rearrange("p (a h) -> p a h", a=2).unsqueeze(3).broadcast_to([128, 2, 4, 64]),
                                                        op=ALU.mult), r=[BpO, Brd], w=[By])
            k.op("pool", lambda: nc.gpsimd.tensor_tensor(out=ysq[:], in0=y[:], in1=y[:], op=ALU.mult), r=[By], w=[Bysq])
            k.op("dve", lambda: nc.vector.tensor_reduce(out=ssq[:], in_=ysq[:], axis=AX.X, op=ALU.add), r=[Bysq], w=[Bssq])
            k.op("act", lambda: nc.scalar.activation(out=ssq[:], in_=ssq[:], func=AF.Ln, bias=EPS, scale=1.0 / 64), r=[Bssq], w=[Bssq])
            k.op("act", lambda: nc.scalar.activation(out=ssq[:], in_=ssq[:], func=AF.Exp, scale=-0.5), r=[Bssq], w=[Bssq])
            k.op("pool", lambda: nc.gpsimd.tensor_tensor(out=y[:], in0=y[:], in1=ssq[:].unsqueeze(2).broadcast_to([128, 8, 64]), op=ALU.mult),
                 r=[By, Bssq], w=[By])
            yb_, Bb = yb[u % 2], Byb[u % 2]
            k.op("pool", lambda: nc.gpsimd.tensor_tensor(out=yb_[:], in0=y[:].rearrange("p h d -> p (h d)"), in1=gna[:], op=ALU.mult),
                 r=[By, Bg], w=[Bb])
            k.dma("sp", T.ymix[u * 128:(u + 1) * 128, 0:512], yb_[:], r=[Bb])

        N = NT * 4
        scores(0)
        for i in range(N):
            if i + 1 < N:
                scores(i + 1)
            softmax_pv(i)
            if i % 4 == 3:
                epilogue(i // 4)


def stage_mlstm(env, layer):
    nc, k, I, T, C, sb, ps = (env[n] for n in ("nc", "k", "I", "T", "C", "sb", "ps"))
    QS = 128 ** -0.5
    with ExitStack() as stk:
        E1 = [sb(stk, f"M_E1{d}", [128, 256], F32) for d in range(2)]
        E2 = [sb(stk, f"M_E2{d}", [128, 256], F32) for d in range(2)]
        EM = [sb(stk, f"M_EM{d}", [128, 256], F32) for d in range(2)]
        EG = [sb(stk, f"M_EG{d}", [128, 256], F32) for d in range(2)]
        BE = Buf()
        with ExitStack() as s2:
            G = sb(s2, "M_G", [128, 64, 16], F32)
            BG = Buf()
            k.dma("sp", G[:], T.gatesP, w=[BG])
            ex = sb(s2, "M_ex", [128, 256], F32)
            SP = sb(s2, "M_SP", [128, 256], F32)
            U = sb(s2, "M_U", [128, 256], F32)
            Bs = sb(s2, "M_Bs", [128, 256], F32)
            Gs = sb(s2, "M_Gs", [128, 256], F32)
            uT = sb(s2, "M_uT", [128, 2, 128], F32)
            cmT = sb(s2, "M_cmT", [128, 2, 128], F32)
            gT = sb(s2, "M_gT", [128, 2, 128], F32)
            amx = sb(s2, "M_amx", [128, 2], F32)
            ngT = sb(s2, "M_ngT", [128, 2], F32)
            cmr = sb(s2, "M_cmr", [128, 256], F32)
            cm = sb(s2, "M_cm", [128, 256], F32)
            MP = sb(s2, "M_MP", [128, 256], F32)
            mx = sb(s2, "M_mx", [128, 256], F32)
            am4 = sb(s2, "M_am4", [4, 64], F32)
            gg4 = sb(s2, "M_gg4", [4, 64], F32)
            m4 = sb(s2, "M_m4", [4, 64], F32)
            mp4 = sb(s2, "M_mp4", [4, 64], F32)
            pA = ps(s2, "M_pA", [128, 256], F32)
            pB = ps(s2, "M_pB", [128, 256], F32)
            pC = ps(s2, "M_pC", [128, 2, 128], F32)
            pD = ps(s2, "M_pD", [128, 2, 128], F32)
            Bx = {n: Buf() for n in ("ex", "SP", "U", "Bs", "Gs", "uT", "cmT", "gT", "amx", "ngT", "cmr", "cm", "MP", "mx",
                                     "am4", "gg4", "m4", "mp4", "pA", "pB", "pC", "pD", "sc")}
            for d in range(2):
                Iv = G[:, :, 8 * d:8 * d + 4]
                Fv = G[:, :, 8 * d + 4:8 * d + 8]
                v3 = lambda t: t[:].rearrange("p (c h) -> p c h", h=4)
                Ud = C.ufw if d == 0 else C.ubw
                idm = C.identf if d == 0 else C.antif
                k.op("act", lambda: nc.scalar.activation(out=v3(ex), in_=Fv, func=AF.Exp, scale=-1.0), r=[BG], w=[Bx["ex"]])
                k.op("act", lambda: nc.scalar.activation(out=SP[:], in_=ex[:], func=AF.Ln, bias=1.0), r=[Bx["ex"]], w=[Bx["SP"]])
                k.op("pe", lambda: nc.tensor.matmul(out=pA[:], lhsT=Ud[:], rhs=SP[:], start=True, stop=True), r=[Bx["SP"], C.B], w=[Bx["pA"]])
                k.op("pe", lambda: nc.tensor.matmul(out=pB[:], lhsT=C.ones[:], rhs=SP[:], start=True, stop=True), r=[Bx["SP"], C.B], w=[Bx["pB"]])
                k.op("dve", lambda: nc.vector.tensor_tensor(out=v3(U), in0=pA[:].rearrange("p (c h) -> p c h", h=4), in1=Iv, op=ALU.add),
                     r=[Bx["pA"], BG], w=[Bx["U"]])
                k.op("dve", lambda: nc.vector.tensor_copy(out=Bs[:], in_=pA[:]), r=[Bx["pA"]], w=[Bx["Bs"]])
                k.op("dve", lambda: nc.vector.tensor_copy(out=Gs[:], in_=pB[:]), r=[Bx["pB"]], w=[Bx["Gs"]])
                k.op("act", lambda: nc.scalar.activation(out=EG[d][:], in_=Gs[:], func=AF.Exp, scale=-1.0), r=[Bx["Gs"]], w=[BE])
                k.op("act", lambda: nc.scalar.activation(out=E1[d][:], in_=U[:], func=AF.Exp), r=[Bx["U"]], w=[BE])
                for blk in range(2):
                    k.op("pe", lambda: nc.tensor.transpose(out=pC[:, blk, :], in_=U[:, blk * 128:(blk + 1) * 128], identity=idm[:]),
                         r=[Bx["U"], C.B], w=[Bx["pC"]])
                    k.op("pe", lambda: nc.tensor.transpose(out=pD[:, blk, :], in_=Gs[:, blk * 128:(blk + 1) * 128], identity=C.identf[:]),
                         r=[Bx["Gs"], C.B], w=[Bx["pD"]])
                k.op("dve", lambda: nc.vector.tensor_copy(out=uT[:], in_=pC[:]), r=[Bx["pC"]], w=[Bx["uT"]])
                k.op("dve", lambda: nc.vector.tensor_copy(out=gT[:], in_=pD[:]), r=[Bx["pD"]], w=[Bx["gT"]])
                for blk in range(2):
                    k.op("dve", lambda: nc.vector.tensor_tensor_scan(out=cmT[:, blk, :], data0=uT[:, blk, :], data1=uT[:, blk, :],
                                                                      initial=-3.0e38, op0=ALU.max, op1=ALU.max),
                         r=[Bx["uT"]], w=[Bx["cmT"]])
                k.op("dve", lambda: nc.vector.tensor_tensor(out=amx[:], in0=cmT[:, :, 127], in1=gT[:, :, 0], op=ALU.subtract),
                     r=[Bx["cmT"], Bx["gT"]], w=[Bx["amx"]])
                k.op("dve", lambda: nc.vector.tensor_scalar(out=ngT[:], in0=gT[:, :, 0], scalar1=-1.0, scalar2=None, op0=ALU.mult),
                     r=[Bx["gT"]], w=[Bx["ngT"]])
                for blk in range(2):
                    k.dma("sp", T.sc[d * 4 + 0:d * 4 + 1, blk * 128:(blk + 1) * 128].rearrange("o n -> n o"), amx[:, blk:blk + 1],
                          r=[Bx["amx"]], w=[Bx["sc"]])
                    k.dma("sp", T.sc[d * 4 + 1:d * 4 + 2, blk * 128:(blk + 1) * 128].rearrange("o n -> n o"), ngT[:, blk:blk + 1],
                          r=[Bx["ngT"]], w=[Bx["sc"]])
                k.dma("sp", am4[:], T.sc[d * 4 + 0].rearrange("(c h) -> h c", h=4), r=[Bx["sc"]], w=[Bx["am4"]],
                      allow_slow_non_contiguous=True)
                k.dma("sp", gg4[:], T.sc[d * 4 + 1].rearrange("(c h) -> h c", h=4), r=[Bx["sc"]], w=[Bx["gg4"]],
                      allow_slow_non_contiguous=True)
                if d == 0:
                    k.op("dve", lambda: nc.vector.tensor_tensor_scan(out=m4[:], data0=gg4[:], data1=am4[:], initial=0.0,
                                                                      op0=ALU.add, op1=ALU.max),
                         r=[Bx["gg4"], Bx["am4"]], w=[Bx["m4"]])
                    k.op("dve", lambda: nc.vector.memset(mp4[:, 0:1], 0.0), w=[Bx["mp4"]])
                    k.op("dve", lambda: nc.vector.tensor_copy(out=mp4[:, 1:64], in_=m4[:, 0:63]), r=[Bx["m4"]], w=[Bx["mp4"]])
                else:
                    k.op("dve", lambda: nc.vector.tensor_tensor(out=m4[:, 63:64], in0=gg4[:, 63:64], in1=am4[:, 63:64], op=ALU.max),
                         r=[Bx["gg4"], Bx["am4"]], w=[Bx["m4"]])
                    for c in range(62, -1, -1):
                        k.op("dve", lambda: nc.vector.scalar_tensor_tensor(out=m4[:, c:c + 1], in0=m4[:, c + 1:c + 2], scalar=gg4[:, c:c + 1],
                                                                            in1=am4[:, c:c + 1], op0=ALU.add, op1=ALU.max),
                             r=[Bx["m4"], Bx["gg4"], Bx["am4"]], w=[Bx["m4"]])
                    k.op("dve", lambda: nc.vector.memset(mp4[:, 63:64], 0.0), w=[Bx["mp4"]])
                    k.op("dve", lambda: nc.vector.tensor_copy(out=mp4[:, 0:63], in_=m4[:, 1:64]), r=[Bx["m4"]], w=[Bx["mp4"]])
                k.dma("sp", T.sc[d * 4 + 2].rearrange("(c h) -> h c", h=4), mp4[:], r=[Bx["mp4"]], w=[Bx["sc"]],
                      allow_slow_non_contiguous=True)
                k.dma("sp", MP[:], T.sc[d * 4 + 2:d * 4 + 3, :].broadcast_to([128, 256]), r=[Bx["sc"]], w=[Bx["MP"]])
                for blk in range(2):
                    k.op("pe", lambda: nc.tensor.transpose(out=pA[:, blk * 128:(blk + 1) * 128], in_=cmT[:, blk, :], identity=C.identf[:]),
                         r=[Bx["cmT"], C.B], w=[Bx["pA"]])
                if d == 0:
                    k.op("dve", lambda: nc.vector.tensor_copy(out=cm[:], in_=pA[:]), r=[Bx["pA"]], w=[Bx["cm"]])
                else:
                    k.op("dve", lambda: nc.vector.tensor_copy(out=cmr[:], in_=pA[:]), r=[Bx["pA"]], w=[Bx["cmr"]])
                    k.op("pe", lambda: nc.tensor.matmul(out=pB[:], lhsT=C.antif[:], rhs=cmr[:], start=True, stop=True),
                         r=[Bx["cmr"], C.B], w=[Bx["pB"]])
                    k.op("dve", lambda: nc.vector.tensor_copy(out=cm[:], in_=pB[:]), r=[Bx["pB"]], w=[Bx["cm"]])
                k.op("dve", lambda: nc.vector.tensor_tensor(out=mx[:], in0=MP[:], in1=cm[:], op=ALU.max), r=[Bx["MP"], Bx["cm"]], w=[Bx["mx"]])
                k.op("act", lambda: nc.scalar.activation(out=E2[d][:], in_=mx[:], func=AF.Exp, scale=-1.0), r=[Bx["mx"]], w=[BE])
                k.op("dve", lambda: nc.vector.tensor_tensor(out=mx[:], in0=Bs[:], in1=mx[:], op=ALU.subtract), r=[Bx["Bs"], Bx["mx"]], w=[Bx["mx"]])
                k.op("act", lambda: nc.scalar.activation(out=EM[d][:], in_=mx[:], func=AF.Exp), r=[Bx["mx"]], w=[BE])
            k.barrier()
        cw = sb(stk, "M_cw", [128, 8, 5], F32)
        cb = sb(stk, "M_cb", [128, 8], F32)
        gml = sb(stk, "M_gml", [128, 512], F32)
        Bcw = Buf()
        for j in range(5):
            k.dma("sp", cw[:, :, j], I.conv_w[layer, j].rearrange("(n p) -> p n", p=128), w=[Bcw], allow_slow_non_contiguous=True)
        k.dma("sp", cb[:], I.conv_b[layer].rearrange("(n p) -> p n", p=128), w=[Bcw], allow_slow_non_contiguous=True)
        k.dma("sp", gml[:], I.g_ml[layer:layer + 1, :].broadcast_to([128, 512]), w=[Bcw])
        Bmask = Buf()
        qT = sb(stk, "M_qT", [128, 2, S], BF16)
        kT = sb(stk, "M_kT", [128, 2, S], BF16)
        BqT = [[Buf() for _ in range(8)] for _ in range(2)]
        BkT = [[Buf() for _ in range(8)] for _ in range(2)]
        va = sb(stk, "M_va", [128, 64, 2, 129], BF16)
        Bva = Buf()
        hacc = sb(stk, "M_hacc", [128, 64, 256], F32)
        Bh = [Buf() for _ in range(64)]
        xin = [sb(stk, f"M_xin{i}", [128, 1028], BF16) for i in range(2)]
        dg = sb(stk, "M_dg", [128, 4, 5, 128], BF16)
        Bdg = Buf()
        Bxin = [Buf(), Buf()]
        V = nc.vector
        Cs = sb(stk, "M_Cs", [128, 4, 129], F32)
        Cb = sb(stk, "M_Cb", [128, 4, 129], BF16)
        BCs, BCb = Buf(), Buf()
        mask4 = sb(stk, "M_mask4", [128, 4, 128], F32)
        for u_ in range(4):
            k.op("act", lambda: nc.scalar.mul(out=mask4[:, u_, :], in_=(C.ufw if u_ < 2 else C.ubw)[:], mul=QS), r=[C.B], w=[Bmask])
        ST = [sb(stk, f"M_ST{i}", [128, 4, 128], BF16) for i in range(2)]
        vp = [sb(stk, f"M_vp{i}", [128, 2, 2, 129], BF16) for i in range(2)]
        nd = [sb(stk, f"M_nd{i}", [128, 2, 2, 129], F32) for i in range(2)]
        kk = [sb(stk, f"M_kk{i}", [128, 4, 128], BF16) for i in range(2)]
        dd = [sb(stk, f"M_dd{i}", [128, 8], F32) for i in range(2)]
        htmp = [sb(stk, "M_htmp", [128, 2, 2, 128], F32)] * 2
        BST, Bvp, Bnd, Bkk, Bdd = ([Buf(), Buf()] for _ in range(5))
        Bhtmp = [Buf()] * 2
        P1 = [ps(stk, f"M_P1{i}", [128, 4, 128], F32) for i in range(2)]
        PT = [ps(stk, f"M_PT{i}", [128, 4, 128], BF16) for i in range(2)]
        P2 = [ps(stk, f"M_P2{d}", [128, 2, 129], F32) for d in range(2)]
        P3 = [ps(stk, f"M_P3{d}", [128, 2, 129], F32) for d in range(2)]
        BP1, BPT, BP2, BP3 = ([Buf(), Buf()] for _ in range(4))
        fsq = sb(stk, "M_fsq", [128, 256], F32)
        fss = sb(stk, "M_fss", [128, 2], F32)
        fy = sb(stk, "M_fy", [128, 256], F32)
        fso = [sb(stk, f"M_fso{i}", [128, 256], BF16) for i in range(2)]
        fyb = [sb(stk, f"M_fyb{i}", [128, 256], BF16) for i in range(2)]
        Bfsq, Bfss, Bfy, Bfso, Bfyb = Buf(), Buf(), Buf(), [Buf(), Buf()], [Buf(), Buf()]
        vsrc = T.vml.rearrange("(c p) (h d) -> p c h d", p=128, d=128)
        ncv = 0
        ncp = 0
        un = 0
        for hp in range(2):
            k.op("pool", lambda: nc.gpsimd.memset(va[:], 1.0), w=[Bva])
            for hh in range(2):
                k.dma("sp", va[:, :, hh, 0:128], vsrc[:, :, hp * 2 + hh, :], w=[Bva])
            for isk_ in range(2):
                for hh_ in range(2):
                    n_ = isk_ * 4 + hp * 2 + hh_
                    for j_ in range(5):
                        k.op("pool", lambda: nc.gpsimd.tensor_scalar(out=dg[:, isk_ * 2 + hh_, j_, :], in0=C.identf[:], scalar1=cw[:, n_, j_:j_ + 1],
                                                                     scalar2=None, op0=ALU.mult), r=[C.B, Bcw], w=[Bdg])
            for isk in range(2):
                for hh in range(2):
                    n = isk * 4 + hp * 2 + hh
                    dstT = kT if isk else qT
                    for pc in range(8):
                        xi, Bxi = xin[ncv % 2], Bxin[ncv % 2]
                        ncv += 1
                        lo = pc * 1024 - 2
                        hi = pc * 1024 + 1026
                        o0 = 0
                        if pc == 0:
                            k.op("pool", lambda: nc.gpsimd.memset(xi[:, 0:2], 0.0), w=[Bxi])
                            lo, o0 = 0, 2
                        if pc == 7:
                            k.op("pool", lambda: nc.gpsimd.memset(xi[:, 1026:1028], 0.0), w=[Bxi])
                            hi = S
                        k.dma("sp", xi[:, o0:o0 + (hi - lo)], T.qkT[n * 128:(n + 1) * 128, lo:hi], w=[Bxi])
                        Bd = (BkT if isk else BqT)[hh][pc]
                        for sub in range(2):
                            pcv = P1[ncp % 2][:].rearrange("p a b -> p (a b)")
                            Bpcv = BP1[ncp % 2]
                            ncp += 1
                            for j in range(5):
                                k.op("pe", lambda: nc.tensor.matmul(out=pcv, lhsT=dg[:, isk * 2 + hh, j, :], rhs=xi[:, sub * 512 + j:sub * 512 + j + 512],
                                                                    start=(j == 0), stop=(j == 4)), r=[Bdg, Bxi], w=[Bpcv])
                            t0_ = pc * 1024 + sub * 512
                            k.op("act", lambda: nc.scalar.activation(out=dstT[:, hh, t0_:t0_ + 512], in_=pcv, func=AF.Silu,
                                                                     bias=cb[:, n:n + 1]), r=[Bpcv, Bcw], w=[Bd])
            for step in range(64):
                i2 = step % 2
                first = (step == 0)
                cc = (step, 63 - step)
                cols = [cc[d] * 4 + hp * 2 for d in range(2)]
                csl = [slice(cc[d] * 128, (cc[d] + 1) * 128) for d in range(2)]
                Bq = [[BqT[hh][cc[d] // 8] for hh in range(2)] for d in range(2)]
                Bk = [[BkT[hh][cc[d] // 8] for hh in range(2)] for d in range(2)]
                for d in range(2):
                    for hh in range(2):
                        k.op("pe", lambda: nc.tensor.matmul(out=P1[i2][:, d * 2 + hh, :], lhsT=kT[:, hh, csl[d]], rhs=qT[:, hh, csl[d]],
                                                            start=True, stop=True), r=[Bq[d][hh], Bk[d][hh]], w=[BP1[i2]])
                k.op("dve", lambda: V.tensor_tensor(out=ST[i2][:], in0=P1[i2][:], in1=mask4[:], op=ALU.mult), r=[BP1[i2], Bmask], w=[BST[i2]])
                for d in range(2):
                    k.op("pool", lambda: nc.gpsimd.tensor_tensor(out=vp[i2][:, d, :, :], in0=va[:, cc[d], :, :],
                                                                 in1=E1[d][:, cols[d]:cols[d] + 2].unsqueeze(2).broadcast_to([128, 2, 129]),
                                                                 op=ALU.mult), r=[Bva, BE], w=[Bvp[i2]])
                for d in range(2):
                    for hh in range(2):
                        k.op("pe", lambda: nc.tensor.matmul(out=P2[d][:, hh, :], lhsT=ST[i2][:, d * 2 + hh, :], rhs=vp[i2][:, d, hh, :],
                                                            start=True, stop=first), r=[BST[i2], Bvp[i2]], w=[BP2[d]])
                        if not first:
                            k.op("pe", lambda: nc.tensor.matmul(out=P2[d][:, hh, :], lhsT=qT[:, hh, csl[d]], rhs=Cb[:, d * 2 + hh, :],
                                                                start=False, stop=True), r=[Bq[d][hh], BCb], w=[BP2[d]])
                for d in range(2):
                    k.op("dve", lambda: V.tensor_tensor(out=nd[i2][:, d, :, :], in0=P2[d][:],
                                                        in1=E2[d][:, cols[d]:cols[d] + 2].unsqueeze(2).broadcast_to([128, 2, 129]), op=ALU.mult),
                         r=[BP2[d], BE], w=[Bnd[i2]])
                den = nd[i2][:, :, :, 128]
                dd_ = dd[i2]
                k.op("dve", lambda: V.scalar_tensor_tensor(out=dd_[:, 0:4].rearrange("p (a b) -> p a b", a=2), in0=den, scalar=-1.0, in1=den,
                                                            op0=ALU.mult, op1=ALU.max), r=[Bnd[i2]], w=[Bdd[i2]])
                for d in range(2):
                    k.op("dve", lambda: V.tensor_tensor(out=dd_[:, 4 + 2 * d:6 + 2 * d], in0=dd_[:, 2 * d:2 * d + 2],
                                                        in1=EM[d][:, cols[d]:cols[d] + 2], op=ALU.max), r=[Bdd[i2], BE], w=[Bdd[i2]])
                k.op("dve", lambda: V.reciprocal(out=dd_[:, 0:4], in_=dd_[:, 4:8]), r=[Bdd[i2]], w=[Bdd[i2]])
                for d in range(2):
                    c = cc[d]
                    hdst = hacc[:, c, :].rearrange("p (h e) -> p h e", h=2)
                    rdb = dd_[:, 2 * d:2 * d + 2].unsqueeze(2).broadcast_to([128, 2, 128])
                    if (d == 0 and c < 32) or (d == 1 and c >= 32):
                        k.op("dve", lambda: V.tensor_tensor(out=hdst, in0=nd[i2][:, d, :, 0:128], in1=rdb, op=ALU.mult),
                             r=[Bnd[i2], Bdd[i2]], w=[Bh[c]])
                    else:
                        k.op("pool", lambda: nc.gpsimd.tensor_tensor(out=htmp[i2][:, d, :, :], in0=nd[i2][:, d, :, 0:128], in1=rdb, op=ALU.mult),
                             r=[Bnd[i2], Bdd[i2]], w=[Bhtmp[i2]])
                        k.op("pool", lambda: nc.gpsimd.tensor_tensor(out=hdst, in0=hdst, in1=htmp[i2][:, d, :, :], op=ALU.add),
                             r=[Bhtmp[i2], Bh[c]], w=[Bh[c]])
                if step == 63:
                    continue
                for d in range(2):
                    for hh in range(2):
                        k.op("pe", lambda: nc.tensor.transpose(out=PT[i2][:, d * 2 + hh, :], in_=kT[:, hh, csl[d]], identity=C.identb[:]),
                             r=[Bk[d][hh], C.B], w=[BPT[i2]])
                k.op("act", lambda: nc.scalar.copy(out=kk[i2][:], in_=PT[i2][:]), r=[BPT[i2]], w=[Bkk[i2]])
                for d in range(2):
                    for hh in range(2):
                        k.op("pe", lambda: nc.tensor.matmul(out=P3[d][:, hh, :], lhsT=kk[i2][:, d * 2 + hh, :], rhs=vp[i2][:, d, hh, :],
                                                            start=True, stop=True), r=[Bkk[i2], Bvp[i2]], w=[BP3[d]])
                for d in range(2):
                    egb = EG[d][:, cols[d]:cols[d] + 2].unsqueeze(2).broadcast_to([128, 2, 129])
                    if first:
                        k.op("dve", lambda: V.tensor_tensor(out=Cs[:, 2 * d:2 * d + 2, :], in0=P3[d][:], in1=egb, op=ALU.mult),
                             r=[BP3[d], BE], w=[BCs])
                    else:
                        k.op("dve", lambda: V.tensor_tensor(out=Cs[:, 2 * d:2 * d + 2, :], in0=Cs[:, 2 * d:2 * d + 2, :], in1=P3[d][:], op=ALU.add),
                             r=[BP3[d], BCs], w=[BCs])
                        k.op("dve", lambda: V.tensor_tensor(out=Cs[:, 2 * d:2 * d + 2, :], in0=Cs[:, 2 * d:2 * d + 2, :], in1=egb, op=ALU.mult),
                             r=[BCs, BE], w=[BCs])
                k.op("act", lambda: nc.scalar.mul(out=Cb[:], in_=Cs[:], mul=QS), r=[BCs], w=[BCb])
            for c in range(64):
                so, Bso = fso[c % 2], Bfso[c % 2]
                ybf, Byb = fyb[c % 2], Bfyb[c % 2]
                k.dma("sp", so[:], T.sigo[c * 128:(c + 1) * 128, hp * 256:(hp + 1) * 256], w=[Bso])
                k.op("pool", lambda: nc.gpsimd.tensor_tensor(out=fsq[:], in0=hacc[:, c, :], in1=hacc[:, c, :], op=ALU.mult), r=[Bh[c]], w=[Bfsq])
                k.op("dve", lambda: nc.vector.tensor_reduce(out=fss[:], in_=fsq[:].rearrange("p (h d) -> p h d", h=2), axis=AX.X, op=ALU.add),
                     r=[Bfsq], w=[Bfss])
                k.op("act", lambda: nc.scalar.activation(out=fss[:], in_=fss[:], func=AF.Sqrt, bias=EPS, scale=1.0 / 128), r=[Bfss], w=[Bfss])
                k.op("dve", lambda: nc.vector.reciprocal(out=fss[:], in_=fss[:]), r=[Bfss], w=[Bfss])
                k.op("dve", lambda: nc.vector.tensor_tensor(out=fy[:].rearrange("p (h d) -> p h d", h=2), in0=hacc[:, c, :].rearrange("p (h d) -> p h d", h=2),
                                                            in1=fss[:].unsqueeze(2).broadcast_to([128, 2, 128]), op=ALU.mult),
                     r=[Bh[c], Bfss], w=[Bfy])
                k.op("pool", lambda: nc.gpsimd.tensor_tensor(out=fy[:], in0=fy[:], in1=gml[:, hp * 256:(hp + 1) * 256], op=ALU.mult),
                     r=[Bfy, Bcw], w=[Bfy])
                k.op("dve", lambda: nc.vector.tensor_tensor(out=ybf[:], in0=fy[:], in1=so[:], op=ALU.mult), r=[Bfy, Bso], w=[Byb])
                k.dma("sp", T.ymix[c * 128:(c + 1) * 128, 512 + hp * 256:512 + (hp + 1) * 256], ybf[:], r=[Byb])
            k.barrier()


def stage_tail(env, layer, src_h, stop_after):
    nc, k, I, T, C, sb, ps, Bh, out, dbg = (env[n] for n in ("nc", "k", "I", "T", "C", "sb", "ps", "Bh", "out", "dbg"))
    BS = 512
    NB = 64
    last = (layer == DEPTH - 1)
    V = nc.vector
    weg, weu, wed = T.Wbf
    with ExitStack() as stk:
        OH1 = sb(stk, "T_OH1", [128, NT, 32], F32)
        OH2 = sb(stk, "T_OH2", [128, NT, 32], F32)
        W12 = sb(stk, "T_W12", [128, NT, 2], F32)
        DI = sb(stk, "T_DI", [128, NT, 2], mybir.dt.int32)
        IG = sb(stk, "T_IG", [128, NB], mybir.dt.int32)
        BOH = Buf()
        with ExitStack() as s2:
            Wo = sb(s2, "T_Wo", [128, 8, D], BF16)
            BWo = Buf()
            k.dma("pool", Wo[:], I.w_out[layer].rearrange("(c p) n -> p c n", p=128), w=[BWo])
            gmoe = sb(s2, "T_gmoe", [128, D], F32)
            brt = sb(s2, "T_brt", [128, 36], F32)
            wr = sb(s2, "T_wr", [128, 8, 36], BF16)
            Bg = Buf()
            k.dma("sp", gmoe[:], I.g_moe[layer:layer + 1, :].broadcast_to([128, D]), w=[Bg])
            k.dma("sp", brt[:], I.b_rt[layer:layer + 1, :].broadcast_to([128, 36]), w=[Bg])
            k.dma("pool", wr[:], I.w_rt[layer].rearrange("(c p) n -> p c n", p=128), w=[Bg])
            for m, src in enumerate((I.w_eg, I.w_eu, I.w_ed)):
                for e in range(32):
                    r0 = (layer * 32 + e) * 128
                    k.dma("pool", T.Wbf[m][e * 128:(e + 1) * 128, :], src[r0:r0 + 128, :], ring="bg")
            ntmps = [make_norm_tmp(env, s2, f"T{i}") for i in range(2)]
            po = [ps(s2, f"T_po{i}", [128, 512], F32) for i in range(2)]
            Bpo = [Buf(), Buf()]
            prs = [ps(s2, f"T_pr{i}", [128, 8, 36], F32) for i in range(2)]
            Bprs = [Buf(), Buf()]
            ym = [sb(s2, f"T_ym{i}", [128, D], BF16) for i in range(3)]
            hb = [sb(s2, f"T_hb{i}", [128, D], F32) for i in range(3)]
            h1 = [sb(s2, f"T_h1{i}", [128, D], F32) for i in range(2)]
            Bym, Bhb, Bh1 = [Buf(), Buf(), Buf()], [Buf(), Buf(), Buf()], [Buf(), Buf()]
            ymTs = [sb(s2, f"T_ymT{i}", [128, 8, 128], BF16) for i in range(2)]
            xTts = [sb(s2, f"T_xTt{i}", [128, 8, 128], BF16) for i in range(2)]
            BymTs, BxTts = [Buf(), Buf()], [Buf(), Buf()]
            Ls = [sb(s2, f"T_L{i}", [128, 8, 36], F32) for i in range(2)]
            rts = [sb(s2, f"T_rt{i}", [128, 8 * 56], F32) for i in range(2)]
            Brts = [Buf(), Buf()]
            npo = 0

            def d_loads(t):
                k.dma("sp", ym[t % 3][:], T.ymix[t * 128:(t + 1) * 128, :], w=[Bym[t % 3]])
                k.dma("sp", hb[t % 3][:], src_h[t * 128:(t + 1) * 128, :], r=[Bh[t]], w=[Bhb[t % 3]])

            def d_stage1(t):
                ntmp = ntmps[t % 2]
                pT, BpT = ntmp["pT"], ntmp["BpT"]
                ymT, BymT = ymTs[t % 2], BymTs[t % 2]
                ymj, Bymj = ym[t % 3], Bym[t % 3]
                hbj, Bhbj = hb[t % 3], Bhb[t % 3]
                h1j, Bh1j = h1[t % 2], Bh1[t % 2]
                for kc in range(8):
                    k.op("pe", lambda: nc.tensor.transpose(out=pT[:, kc * 128:(kc + 1) * 128], in_=ymj[:, kc * 128:(kc + 1) * 128],
                                                           identity=C.identb[:]), r=[Bymj, C.B], w=[BpT])
                k.op("act", lambda: nc.scalar.copy(out=ymT[:], in_=pT[:].rearrange("p (c t) -> p c t", c=8)), r=[BpT], w=[BymT])
                for cg in range(2):
                    p_, Bp_ = po[cg], Bpo[cg]
                    for kc in range(8):
                        k.op("pe", lambda: nc.tensor.matmul(out=p_[:], lhsT=ymT[:, kc, :], rhs=Wo[:, kc, cg * 512:(cg + 1) * 512],
                                                            start=(kc == 0), stop=(kc == 7)), r=[BymT, BWo], w=[Bp_])
                    k.op("dve", lambda: V.tensor_tensor(out=h1j[:, cg * 512:(cg + 1) * 512], in0=p_[:],
                                                        in1=hbj[:, cg * 512:(cg + 1) * 512], op=ALU.add), r=[Bp_, Bhbj], w=[Bh1j])
                k.dma("sp", T.h[t * 128:(t + 1) * 128, :], h1j[:], r=[Bh1j], w=[Bh[t]])
                if "h_mix" in dbg and layer == 0:
                    k.dma("sp", dbg["h_mix"][t * 128:(t + 1) * 128, :], h1j[:], r=[Bh1j])
                norm_pre(env, h1j[:], Bh1j, gmoe, Bg, ntmp)

            d_loads(0)
            d_loads(1)
            d_stage1(0)
            for t in range(NT):
                if t + 2 < NT:
                    d_loads(t + 2)
                if t + 1 < NT:
                    d_stage1(t + 1)
                ntmp = ntmps[t % 2]
                xTt, BxTt = xTts[t % 2], BxTts[t % 2]
                norm_post(env, xTt, BxTt, 0, ntmp)
                k.dma("sp", T.Xn[t * 128:(t + 1) * 128, :], ntmp["a"][:], r=[ntmp["Ba"]])
                RG = 8
                tj = t % RG
                pr, Bpr = prs[(t // RG) % 2], Bprs[(t // RG) % 2]
                for kc in range(8):
                    k.op("pe", lambda: nc.tensor.matmul(out=pr[:, tj, :], lhsT=xTt[:, kc, :], rhs=wr[:, kc, :],
                                                        start=(kc == 0), stop=(kc == 7)), r=[BxTt, Bg], w=[Bpr])
                if tj != RG - 1:
                    continue
                ta = t - (RG - 1)
                L, rt, Brt = Ls[(t // RG) % 2], rts[(t // RG) % 2], Brts[(t // RG) % 2]
                R = [Brt]
                f3 = lambda off, n: rt[:, off:off + RG * n].rearrange("p (a b) -> p a b", a=RG)
                f2 = lambda off: rt[:, off:off + RG]
                bc = lambda ap2, n: ap2.unsqueeze(2).broadcast_to([128, RG, n])
                gmax, gsum, pgt, m1, m2, dm, p2, t1 = (f2(i * RG) for i in range(8))
                o = 8 * RG
                ge, oh, esel, eq1, e2, eq2, tm8 = f3(o, 4), f3(o + 4 * RG, 4), f3(o + 8 * RG, 8), f3(o + 16 * RG, 8), f3(o + 24 * RG, 8), f3(o + 32 * RG, 8), f3(o + 40 * RG, 8)
                w1, w2 = W12[:, ta:t + 1, 0], W12[:, ta:t + 1, 1]
                k.op("dve", lambda: V.tensor_tensor(out=L[:], in0=pr[:], in1=brt[:].unsqueeze(1).broadcast_to([128, RG, 36]), op=ALU.add), r=[Bpr, Bg], w=R)
                k.op("dve", lambda: V.tensor_reduce(out=gmax, in_=L[:, :, 0:4], axis=AX.X, op=ALU.max), r=R, w=R)
                k.op("dve", lambda: V.tensor_tensor(out=ge, in0=L[:, :, 0:4], in1=bc(gmax, 4), op=ALU.subtract), r=R, w=R)
                k.op("act", lambda: nc.scalar.activation(out=ge, in_=ge, func=AF.Exp), r=R, w=R)
                k.op("dve", lambda: V.tensor_reduce(out=gsum, in_=ge, axis=AX.X, op=ALU.add), r=R, w=R)
                k.op("dve", lambda: V.reciprocal(out=pgt, in_=gsum), r=R, w=R)
                k.op("dve", lambda: V.tensor_tensor(out=oh, in0=L[:, :, 0:4], in1=bc(gmax, 4), op=ALU.is_equal), r=R, w=R)
                k.op("dve", lambda: V.tensor_tensor(out=esel, in0=L[:, :, 4:12], in1=oh[:, :, 0:1].broadcast_to([128, RG, 8]), op=ALU.mult), r=R, w=R)
                for gi in range(1, 4):
                    k.op("dve", lambda: V.tensor_tensor(out=tm8, in0=L[:, :, 4 + 8 * gi:12 + 8 * gi], in1=oh[:, :, gi:gi + 1].broadcast_to([128, RG, 8]),
                                                        op=ALU.mult), r=R, w=R)
                    k.op("dve", lambda: V.tensor_tensor(out=esel, in0=esel, in1=tm8, op=ALU.add), r=R, w=R)
                k.op("dve", lambda: V.tensor_reduce(out=m1, in_=esel, axis=AX.X, op=ALU.max), r=R, w=R)
                k.op("dve", lambda: V.tensor_tensor(out=eq1, in0=esel, in1=bc(m1, 8), op=ALU.is_equal), r=R, w=R)
                k.op("dve", lambda: V.scalar_tensor_tensor(out=e2, in0=eq1, scalar=NEG, in1=esel, op0=ALU.mult, op1=ALU.add), r=R, w=R)
                k.op("dve", lambda: V.tensor_reduce(out=m2, in_=e2, axis=AX.X, op=ALU.max), r=R, w=R)
                k.op("dve", lambda: V.tensor_tensor(out=eq2, in0=e2, in1=bc(m2, 8), op=ALU.is_equal), r=R, w=R)
                k.op("dve", lambda: V.tensor_tensor(out=dm, in0=m2, in1=m1, op=ALU.subtract), r=R, w=R)
                k.op("act", lambda: nc.scalar.activation(out=p2, in_=dm, func=AF.Exp), r=R, w=R)
                k.op("dve", lambda: V.tensor_scalar(out=t1, in0=p2, scalar1=1.0, scalar2=None, op0=ALU.add), r=R, w=R)
                k.op("dve", lambda: V.reciprocal(out=t1, in_=t1), r=R, w=R)
                k.op("dve", lambda: V.tensor_tensor(out=w1, in0=t1, in1=pgt, op=ALU.mult), r=R, w=R + [BOH])
                k.op("dve", lambda: V.tensor_tensor(out=w2, in0=w1, in1=p2, op=ALU.mult), r=R + [BOH], w=[BOH])
                for gi in range(4):
                    k.op("dve", lambda: V.tensor_tensor(out=OH1[:, ta:t + 1, 8 * gi:8 * gi + 8], in0=eq1, in1=oh[:, :, gi:gi + 1].broadcast_to([128, RG, 8]),
                                                        op=ALU.mult), r=R, w=[BOH])
                    k.op("dve", lambda: V.tensor_tensor(out=OH2[:, ta:t + 1, 8 * gi:8 * gi + 8], in0=eq2, in1=oh[:, :, gi:gi + 1].broadcast_to([128, RG, 8]),
                                                        op=ALU.mult), r=R, w=[BOH])
            k.barrier()
        if stop_after == ("D", layer):
            return
        with ExitStack() as s2:
            cntp = ps(s2, "T_cntp", [128, 32], F32)
            pR = [ps(s2, f"T_pR{i}", [128, 32], F32) for i in range(2)]
            pC = [ps(s2, f"T_pC{i}", [128, 32], F32) for i in range(2)]
            BpR, BpC = [Buf(), Buf()], [Buf(), Buf()]
            Bc = Buf()
            ustr = sb(s2, "T_ustr", [128, 128], F32)
            k.op("dve", lambda: V.tensor_tensor(out=ustr[:], in0=C.ufw[:], in1=C.identf[:], op=ALU.subtract), r=[C.B], w=[Bc])
            for t in range(NT):
                k.op("pe", lambda: nc.tensor.matmul(out=cntp[:], lhsT=C.ones[:], rhs=OH1[:, t, :], start=(t == 0), stop=False), r=[BOH, C.B], w=[Bc])
                k.op("pe", lambda: nc.tensor.matmul(out=cntp[:], lhsT=C.ones[:], rhs=OH2[:, t, :], start=False, stop=(t == NT - 1)), r=[BOH, C.B], w=[Bc])
            cnt = sb(s2, "T_cnt", [128, 32], F32)
            cmp3 = sb(s2, "T_cmp3", [128, 32, 32], F32)
            nblk = sb(s2, "T_nblk", [128, 32], F32)
            padded = sb(s2, "T_padded", [128, 32], F32)
            zer = sb(s2, "T_zer", [128, 32], F32)
            pend = sb(s2, "T_pend", [128, 32], F32)
            brun = sb(s2, "T_brun", [128, 32], F32)
            eb3 = sb(s2, "T_eb3", [128, NB, 32], F32)
            eb = sb(s2, "T_eb", [128, NB], F32)
            igf = sb(s2, "T_igf", [128, NB], F32)
            df = sb(s2, "T_df", [128, NT, 2], F32)
            dmt = sb(s2, "T_dmt", [128, 32], F32)
            tt = sb(s2, "T_tt", [128, 32], F32)
            R = [Bc]
            k.op("dve", lambda: V.tensor_copy(out=cnt[:], in_=cntp[:]), r=R, w=R)
            k.op("dve", lambda: V.memset(zer[:], 0.0), w=R)
            k.op("dve", lambda: V.tensor_tensor(out=cmp3[:], in0=cnt[:].unsqueeze(2).broadcast_to([128, 32, 32]),
                                                in1=C.jB[:].unsqueeze(1).broadcast_to([128, 32, 32]), op=ALU.is_gt), r=R + [C.B], w=R)
            k.op("dve", lambda: V.tensor_reduce(out=nblk[:], in_=cmp3[:], axis=AX.X, op=ALU.add), r=R, w=R)
            k.op("dve", lambda: V.tensor_scalar(out=padded[:], in0=nblk[:], scalar1=float(BS), scalar2=None, op0=ALU.mult), r=R, w=R)
            k.op("dve", lambda: V.tensor_tensor_scan(out=pend[:], data0=padded[:], data1=zer[:], initial=0.0, op0=ALU.add, op1=ALU.add), r=R, w=R)
            k.op("dve", lambda: V.tensor_tensor(out=brun[:], in0=pend[:], in1=padded[:], op=ALU.subtract), r=R, w=R)
            k.op("dve", lambda: V.tensor_tensor(out=eb3[:], in0=pend[:].unsqueeze(1).broadcast_to([128, NB, 32]),
                                                in1=C.bst[:].unsqueeze(2).broadcast_to([128, NB, 32]), op=ALU.is_le), r=R + [C.B], w=R)
            k.op("dve", lambda: V.tensor_reduce(out=eb[:], in_=eb3[:], axis=AX.X, op=ALU.add), r=R, w=R)
            k.op("dve", lambda: V.tensor_scalar(out=eb[:], in0=eb[:], scalar1=31.0, scalar2=None, op0=ALU.min), r=R, w=R)
            k.op("dve", lambda: V.tensor_scalar(out=igf[:], in0=eb[:], scalar1=128.0, scalar2=C.base8[:, 0:1], op0=ALU.mult, op1=ALU.add), r=R + [C.B], w=R)
            k.op("dve", lambda: V.tensor_copy(out=IG[:], in_=igf[:]), r=R, w=R)
            for t in range(NT):
                pr_, Bpr_ = pR[t % 2], BpR[t % 2]
                pc_, Bpc_ = pC[t % 2], BpC[t % 2]
                k.op("pe", lambda: nc.tensor.matmul(out=pr_[:], lhsT=ustr[:], rhs=OH1[:, t, :], start=True, stop=False), r=[BOH, Bc], w=[Bpr_])
                k.op("pe", lambda: nc.tensor.matmul(out=pr_[:], lhsT=ustr[:], rhs=OH2[:, t, :], start=False, stop=True), r=[BOH, Bc], w=[Bpr_])
                k.op("pe", lambda: nc.tensor.matmul(out=pc_[:], lhsT=C.ones[:], rhs=OH1[:, t, :], start=True, stop=False), r=[BOH, C.B], w=[Bpc_])
                k.op("pe", lambda: nc.tensor.matmul(out=pc_[:], lhsT=C.ones[:], rhs=OH2[:, t, :], start=False, stop=True), r=[BOH, C.B], w=[Bpc_])
                k.op("dve", lambda: V.tensor_tensor(out=dmt[:], in0=pr_[:], in1=brun[:], op=ALU.add), r=R + [Bpr_], w=R)
                k.op("dve", lambda: V.tensor_tensor(out=tt[:], in0=dmt[:], in1=OH1[:, t, :], op=ALU.mult), r=R + [BOH], w=R)
                k.op("dve", lambda: V.tensor_reduce(out=df[:, t, 0:1], in_=tt[:], axis=AX.X, op=ALU.add), r=R, w=R)
                k.op("dve", lambda: V.tensor_tensor(out=tt[:], in0=dmt[:], in1=OH2[:, t, :], op=ALU.mult), r=R + [BOH], w=R)
                k.op("dve", lambda: V.tensor_reduce(out=df[:, t, 1:2], in_=tt[:], axis=AX.X, op=ALU.add), r=R, w=R)
                k.op("dve", lambda: V.tensor_tensor(out=brun[:], in0=brun[:], in1=pc_[:], op=ALU.add), r=R + [Bpc_], w=R)
            k.op("dve", lambda: V.tensor_copy(out=DI[:], in_=df[:]), r=R, w=R)
            if "dest" in dbg and layer == 0:
                k.dma("sp", dbg["dest"], df[:], r=R)
                k.dma("sp", dbg["eb"], eb[:], r=R)
            k.barrier()
        if stop_after == ("R", layer):
            return
        with ExitStack() as s2:
            xs = [sb(s2, f"T_xs{i}", [128, D], BF16) for i in range(3)]
            Bxs = [Buf() for _ in range(3)]
            for t in range(NT):
                x_, Bx_ = xs[t % 3], Bxs[t % 3]
                k.dma("sp", x_[:], T.Xn[t * 128:(t + 1) * 128, :], w=[Bx_])
                for s_ in range(2):
                    k.idma(T.Xs, bass.IndirectOffsetOnAxis(ap=DI[:, t, s_:s_ + 1], axis=0), x_[:], None, r=[Bx_], w=[], bc=NB * BS - 1)
            k.barrier()
        if stop_after == ("S", layer):
            return
        with ExitStack() as s2:
            Wsl = [sb(s2, f"T_Wsl{i}", [128, 12288], BF16) for i in range(2)]
            BW = [Buf(), Buf()]
            xb = [sb(s2, f"T_xb{i}", [128, 4, D], BF16) for i in range(2)]
            xT = [sb(s2, f"T_xT{i}", [128, 8, 512], BF16) for i in range(2)]
            Bxb, BxT = [Buf(), Buf()], [Buf(), Buf()]
            hT = [sb(s2, f"T_hT{i}", [128, 4, 512], BF16) for i in range(2)]
            BhT = [Buf(), Buf()]
            sil = [sb(s2, f"T_sil{i}", [128, 512], F32) for i in range(2)]
            Bsil = [Buf(), Buf()]
            yo = [sb(s2, f"T_yo{i}", [128, D], BF16) for i in range(3)]
            Byo = [Buf() for _ in range(3)]
            pTs = [ps(s2, f"T_EpT{i}", [128, D], BF16) for i in range(2)]
            BpTs = [Buf(), Buf()]
            pg = [ps(s2, f"T_pg{i}", [128, 512], F32) for i in range(2)]
            pu = [ps(s2, f"T_pu{i}", [128, 512], F32) for i in range(2)]
            po = [ps(s2, f"T_Epo{i}", [128, 512], F32) for i in range(2)]
            Bpg, Bpu, Bpo = [Buf(), Buf()], [Buf(), Buf()], [Buf(), Buf()]
            ngu = 0
            npo = 0
            nyo = 0
            def e_loads(b):
                sl = b % 2
                ioff = bass.IndirectOffsetOnAxis(ap=IG[:, b:b + 1], axis=0)
                k.idma(Wsl[sl][:, 0:4096], None, weg, ioff, r=[], w=[BW[sl]])
                k.idma(Wsl[sl][:, 4096:8192], None, weu, ioff, r=[], w=[BW[sl]])
                k.idma(Wsl[sl][:, 8192:12288], None, wed, ioff, r=[], w=[BW[sl]])
                k.dma("sp", xb[sl][:], T.Xs[b * BS:(b + 1) * BS, :].rearrange("(a p) n -> p a n", p=128), w=[Bxb[sl]])

            e_loads(0)
            for b in range(NB):
                sl = b % 2
                if b + 1 < NB:
                    e_loads(b + 1)
                Wt, BWt = Wsl[sl], BW[sl]
                Wg = Wt[:, 0:4096].rearrange("p (c n) -> p c n", c=8)
                Wu = Wt[:, 4096:8192].rearrange("p (c n) -> p c n", c=8)
                Wd = Wt[:, 8192:12288].rearrange("p (c n) -> p c n", c=4)
                for a in range(4):
                    pT, BpT = pTs[a % 2], BpTs[a % 2]
                    for kc in range(8):
                        k.op("pe", lambda: nc.tensor.transpose(out=pT[:, kc * 128:(kc + 1) * 128], in_=xb[sl][:, a, kc * 128:(kc + 1) * 128],
                                                               identity=C.identb[:]), r=[Bxb[sl], C.B], w=[BpT])
                    cp = (lambda: nc.scalar.copy(out=xT[sl][:, :, a * 128:(a + 1) * 128], in_=pT[:].rearrange("p (c t) -> p c t", c=8))) if a % 2 == 0 else \
                         (lambda: V.tensor_copy(out=xT[sl][:, :, a * 128:(a + 1) * 128], in_=pT[:].rearrange("p (c t) -> p c t", c=8)))
                    k.op("act" if a % 2 == 0 else "dve", cp, r=[BpT], w=[BxT[sl]])
                hT_, BhT_ = hT[sl], BhT[sl]
                for m in range(4):
                    pg_, Bpg_ = pg[ngu % 2], Bpg[ngu % 2]
                    pu_, Bpu_ = pu[ngu % 2], Bpu[ngu % 2]
                    sl_, Bsl_ = sil[ngu % 2], Bsil[ngu % 2]
                    ngu += 1
                    for kc in range(8):
                        k.op("pe", lambda: nc.tensor.matmul(out=pg_[:], lhsT=Wg[:, kc, m * 128:(m + 1) * 128], rhs=xT[sl][:, kc, :],
                                                            start=(kc == 0), stop=(kc == 7)), r=[BWt, BxT[sl]], w=[Bpg_])
                    for kc in range(8):
                        k.op("pe", lambda: nc.tensor.matmul(out=pu_[:], lhsT=Wu[:, kc, m * 128:(m + 1) * 128], rhs=xT[sl][:, kc, :],
                                                            start=(kc == 0), stop=(kc == 7)), r=[BWt, BxT[sl]], w=[Bpu_])
                    k.op("act", lambda: nc.scalar.activation(out=sl_[:], in_=pg_[:], func=AF.Silu), r=[Bpg_], w=[Bsl_])
                    k.op("dve", lambda: V.tensor_tensor(out=hT_[:, m, :], in0=sl_[:], in1=pu_[:], op=ALU.mult), r=[Bsl_, Bpu_], w=[BhT_])
                for jj in range(4):
                    yo_, Byo_ = yo[nyo % 3], Byo[nyo % 3]
                    nyo += 1
                    for cg in range(2):
                        p_, Bp_ = po[npo % 2], Bpo[npo % 2]
                        npo += 1
                        for m in range(4):
                            k.op("pe", lambda: nc.tensor.matmul(out=p_[:], lhsT=hT_[:, m, jj * 128:(jj + 1) * 128],
                                                                rhs=Wd[:, m, cg * 512:(cg + 1) * 512], start=(m == 0), stop=(m == 3)),
                                 r=[BhT_, BWt], w=[Bp_])
                        if cg == 0:
                            k.op("act", lambda: nc.scalar.copy(out=yo_[:, 0:512], in_=p_[:]), r=[Bp_], w=[Byo_])
                        else:
                            k.op("dve", lambda: V.tensor_copy(out=yo_[:, 512:1024], in_=p_[:]), r=[Bp_], w=[Byo_])
                    k.dma("sp", T.Ys[b * BS + jj * 128:b * BS + (jj + 1) * 128, :], yo_[:], r=[Byo_])
            k.barrier()
        if stop_after == ("E", layer):
            return
        with ExitStack() as s2:
            Wpg = sb(s2, "T_Wpg", [128, 8, D], BF16)
            Wple = sb(s2, "T_Wple", [128, 2, D], BF16)
            BWp = Buf()
            k.dma("pool", Wpg[:], I.w_pg[layer].rearrange("(c p) n -> p c n", p=128), w=[BWp])
            k.dma("pool", Wple[:], I.w_ple[layer].rearrange("(c p) n -> p c n", p=128), w=[BWp])
            gple = sb(s2, "T_gple", [128, D], F32)
            gfin = sb(s2, "T_gfin", [128, D], F32)
            Bg = Buf()
            k.dma("sp", gple[:], I.g_ple[layer:layer + 1, :].broadcast_to([128, D]), w=[Bg])
            k.dma("sp", gfin[:], I.g_final[0:1, :].broadcast_to([128, D]), w=[Bg])
            ntmps = [make_norm_tmp(env, s2, f"F{i}") for i in range(2)]
            pg = [ps(s2, f"T_Fpg{i}", [128, 512], F32) for i in range(2)]
            pu = [ps(s2, f"T_Fpu{i}", [128, 512], F32) for i in range(2)]
            Bpg, Bpu = [Buf(), Buf()], [Buf(), Buf()]
            y1 = [sb(s2, f"T_y1{i}", [128, D], BF16) for i in range(3)]
            y2 = [sb(s2, f"T_y2{i}", [128, D], BF16) for i in range(3)]
            hb = [sb(s2, f"T_Fhb{i}", [128, D], F32) for i in range(3)]
            By1, By2, Bhb = ([Buf() for _ in range(3)] for _ in range(3))
            aTps = [sb(s2, f"T_aTp{i}", [128, 8, 128], BF16) for i in range(2)]
            BaTps = [Buf(), Buf()]
            sigs = [sb(s2, f"T_sig{i}", [128, D], F32) for i in range(2)]
            Bsigs = [Buf(), Buf()]
            h2 = [sb(s2, f"T_h2{i}", [128, D], F32) for i in range(2)]
            Bh2 = [Buf(), Buf()]
            pins = [sb(s2, f"T_pin{i}", [128, 256], F32) for i in range(3)]
            pbfs = [sb(s2, f"T_pbf{i}", [128, 256], BF16) for i in range(2)]
            ppTs = [sb(s2, f"T_ppT{i}", [128, 2, 128], BF16) for i in range(2)]
            Bpins, Bpbfs, BppTs = [Buf(), Buf(), Buf()], [Buf(), Buf()], [Buf(), Buf()]
            fsss = [sb(s2, f"T_fss{i}", [128, 2], F32) for i in range(2)]
            Bfsss = [Buf(), Buf()]
            def f_loads(t):
                j2 = t % 3
                k.idma(y1[j2][:], None, T.Ys, bass.IndirectOffsetOnAxis(ap=DI[:, t, 0:1], axis=0), r=[], w=[By1[j2]])
                k.idma(y2[j2][:], None, T.Ys, bass.IndirectOffsetOnAxis(ap=DI[:, t, 1:2], axis=0), r=[], w=[By2[j2]])
                k.dma("sp", hb[j2][:], T.h[t * 128:(t + 1) * 128, :], r=[Bh[t]], w=[Bhb[j2]])
                k.dma("sp", pins[j2][:], I.p[layer, t * 128:(t + 1) * 128, :], w=[Bpins[j2]])

            def f_stage1(t):
                i2 = t % 2
                i3 = t % 3
                ntmp = ntmps[i2]
                pT, BpT = ntmp["pT"], ntmp["BpT"]
                k.op("dve", lambda: V.scalar_tensor_tensor(out=hb[i3][:], in0=y1[i3][:], scalar=W12[:, t, 0:1], in1=hb[i3][:],
                                                            op0=ALU.mult, op1=ALU.add), r=[By1[i3], Bhb[i3], BOH], w=[Bhb[i3]])
                k.op("dve", lambda: V.scalar_tensor_tensor(out=hb[i3][:], in0=y2[i3][:], scalar=W12[:, t, 1:2], in1=hb[i3][:],
                                                            op0=ALU.mult, op1=ALU.add), r=[By2[i3], Bhb[i3], BOH], w=[Bhb[i3]])
                if "h_moe" in dbg and layer == 0:
                    k.dma("sp", dbg["h_moe"][t * 128:(t + 1) * 128, :], hb[i3][:], r=[Bhb[i3]])
                norm_pre(env, hb[i3][:], Bhb[i3], gple, Bg, ntmp)
                k.op("dve", lambda: V.tensor_copy(out=pbfs[i2][:], in_=pins[i3][:]), r=[Bpins[i3]], w=[Bpbfs[i2]])
                norm_post(env, aTps[i2], BaTps[i2], 0, ntmp)
                for kc in range(2):
                    k.op("pe", lambda: nc.tensor.transpose(out=pT[:, kc * 128:(kc + 1) * 128], in_=pbfs[i2][:, kc * 128:(kc + 1) * 128],
                                                           identity=C.identb[:]), r=[Bpbfs[i2], C.B], w=[BpT])
                k.op("act", lambda: nc.scalar.copy(out=ppTs[i2][:], in_=pT[:, 0:256].rearrange("p (c t) -> p c t", c=2)), r=[BpT], w=[BppTs[i2]])

            def f_stage2(t):
                i2 = t % 2
                aTp, BaTp, sig, Bsig = aTps[i2], BaTps[i2], sigs[i2], Bsigs[i2]
                ppT, BppT = ppTs[i2], BppTs[i2]
                fss, Bfss = fsss[i2], Bfsss[i2]
                h2_, Bh2_ = h2[i2], Bh2[i2]
                for cg in range(2):
                    cs_ = slice(cg * 512, (cg + 1) * 512)
                    pg_, Bpg_ = pg[cg], Bpg[cg]
                    pu_, Bpu_ = pu[cg], Bpu[cg]
                    for kc in range(8):
                        k.op("pe", lambda: nc.tensor.matmul(out=pg_[:], lhsT=aTp[:, kc, :], rhs=Wpg[:, kc, cs_], start=(kc == 0), stop=(kc == 7)),
                             r=[BaTp, BWp], w=[Bpg_])
                    k.op("act", lambda: nc.scalar.activation(out=sig[:, cs_], in_=pg_[:], func=AF.Sigmoid), r=[Bpg_], w=[Bsig])
                    for kc in range(2):
                        k.op("pe", lambda: nc.tensor.matmul(out=pu_[:], lhsT=ppT[:, kc, :], rhs=Wple[:, kc, cs_], start=(kc == 0), stop=(kc == 1)),
                             r=[BppT, BWp], w=[Bpu_])
                    k.op("dve", lambda: V.tensor_tensor(out=h2_[:, cs_], in0=pu_[:], in1=sig[:, cs_], op=ALU.mult), r=[Bpu_, Bsig], w=[Bh2_])
                k.op("dve", lambda: V.tensor_tensor(out=h2_[:], in0=h2_[:], in1=hb[t % 3][:], op=ALU.add), r=[Bh2_, Bhb[t % 3]], w=[Bh2_])
                if not last:
                    k.dma("sp", T.h[t * 128:(t + 1) * 128, :], h2_[:], r=[Bh2_], w=[Bh[t]])
                    if "h_ple" in dbg and layer == 0:
                        k.dma("sp", dbg["h_ple"][t * 128:(t + 1) * 128, :], h2_[:], r=[Bh2_])
                else:
                    k.op("act", lambda: nc.scalar.activation(out=sig[:], in_=h2_[:], func=AF.Square, accum_out=fss[:, 0:1]), r=[Bh2_], w=[Bsig, Bfss])
                    k.op("act", lambda: nc.scalar.activation(out=fss[:, 1:2], in_=fss[:, 0:1], func=AF.Ln, bias=EPS, scale=1.0 / D), r=[Bfss], w=[Bfss])
                    k.op("act", lambda: nc.scalar.activation(out=fss[:, 1:2], in_=fss[:, 1:2], func=AF.Exp, scale=-0.5), r=[Bfss], w=[Bfss])
                    k.op("dve", lambda: V.scalar_tensor_tensor(out=h2_[:], in0=h2_[:], scalar=fss[:, 1:2], in1=gfin[:], op0=ALU.mult, op1=ALU.mult),
                         r=[Bh2_, Bfss, Bg], w=[Bh2_])
                    k.dma("sp", out[t * 128:(t + 1) * 128, :], h2_[:], r=[Bh2_])

            f_loads(0)
            f_loads(1)
            f_stage1(0)
            for t in range(NT):
                if t + 2 < NT:
                    f_loads(t + 2)
                if t + 1 < NT:
                    f_stage1(t + 1)
                f_stage2(t)
            k.barrier()


def _prep_inputs(inputs):
    f = lambda a: np.ascontiguousarray(np.asarray(a, dtype=np.float32))
    shared = {}
    for name in ("w_in", "b_gate", "conv_w", "conv_b", "g_na", "g_ml", "w_out", "g_mix", "g_moe",
                 "g_ple", "w_ple", "w_ple_gate"):
        shared[name] = f(inputs[name])
    for name, kc in (("w_exp_gate", 8), ("w_exp_up", 8), ("w_exp_down", 4)):
        w = np.asarray(inputs[name], dtype=np.float32)
        n = w.shape[-1]
        w = w.reshape(DEPTH, 32, kc, 128, n).transpose(0, 1, 3, 2, 4)
        shared[name] = np.ascontiguousarray(w).reshape(DEPTH * 32 * 128, kc * n)
    shared["w_rt"] = f(np.concatenate([inputs["w_route_group"], inputs["w_route_expert"]], axis=-1))
    shared["b_rt"] = f(np.concatenate([inputs["b_route_group"], inputs["b_route_expert"]], axis=-1))
    shared["g_final"] = f(inputs["g_final"]).reshape(1, D)
    rpb = f(inputs["rpb"])
    shared["natab"] = np.stack([_na_tables(rpb[l]).reshape(5, 128, 8 * 5 * 128) for l in range(DEPTH)])
    shared.update(_consts())
    x = f(inputs["x"])
    p = f(inputs["p"])
    in_maps = []
    for b in range(8):
        m = dict(shared)
        m["x"] = x[b]
        m["p"] = np.ascontiguousarray(p[:, b])
        in_maps.append(m)
    return in_maps


def kernel(**inputs):
    in_maps = _prep_inputs(inputs)
    nc = build_program()
    res = run_bass_kernel_spmd(nc, in_maps, core_ids=list(range(8)))
    return np.stack([np.asarray(r["out"], dtype=np.float32) for r in res.results], axis=0)
```
